# Optimizing a Trainium2 kernel written in Bass

```python
import jax, jax.numpy as jnp
from jax import lax
import numpy as np

D_MODEL = 1024
BATCH = 2
SEQ = 8192
DEPTH = 1

ROPE_THETA = 500000.0
NORM_EPS = 1e-6
NEG_BIG = -1e30
MLA_HEADS = 8
MLA_NOPE_DIM = 64
MLA_ROPE_DIM = 32
MLA_V_DIM = 64
MLA_Q_RANK = 384
MLA_KV_RANK = 256
MLA_WIDTH = MLA_HEADS * MLA_V_DIM
Q_BLOCK = 128
DIL_HEADS = 8
DIL_HEAD_DIM = 64
DIL_WIDTH = DIL_HEADS * DIL_HEAD_DIM
DIL_ROT_DIM = DIL_HEAD_DIM // 4
DIL_BRANCHES = ((128, 1), (512, 4), (2048, 16))
MIX_WIDTH = MLA_WIDTH + DIL_WIDTH
IN_COLS = MLA_Q_RANK + MLA_KV_RANK + MLA_ROPE_DIM + 3 * DIL_WIDTH
N_EXPERTS = 16
EXPERT_FF = 1024
EC_CAPACITY_FACTOR = 2
N_MOD = 6

kernel_name = "hybrid_mla_dilated_ec_moe_adaln_encoder"


def rms_norm(x, g):
    xf = x.astype(jnp.float32)
    y = xf * lax.rsqrt(jnp.mean(xf * xf, axis=-1, keepdims=True) + NORM_EPS)
    return (y * g.astype(jnp.float32)).astype(x.dtype)


def rope_cos_sin(positions, dim):
    inv = 1.0 / (ROPE_THETA ** (jnp.arange(0, dim, 2, dtype=jnp.float32) / dim))
    ang = positions.astype(jnp.float32)[..., None] * inv
    return jnp.cos(ang), jnp.sin(ang)


def apply_rope(t, cos, sin):
    t1, t2 = jnp.split(t, 2, axis=-1)
    c = cos[:, :, None, :].astype(t.dtype)
    s = sin[:, :, None, :].astype(t.dtype)
    return jnp.concatenate([t1 * c - t2 * s, t1 * s + t2 * c], axis=-1)


def mla_attention(q, k, v):
    B, S, H, Dq = q.shape
    nblk = S // Q_BLOCK
    scale = Dq ** -0.5
    qb = q.reshape(B, nblk, Q_BLOCK, H, Dq).transpose(1, 0, 2, 3, 4)

    def one_block(q_blk):
        s = jnp.einsum('bqhd,bkhd->bhqk', q_blk, k).astype(jnp.float32) * scale
        p = jax.nn.softmax(s, axis=-1).astype(v.dtype)
        return jnp.einsum('bhqk,bkhd->bqhd', p, v)

    o = lax.map(one_block, qb)
    return o.transpose(1, 0, 2, 3, 4).reshape(B, S, H, v.shape[-1])


def dilated_branch(q, k, v, dilation, half):
    B, S, H, Dh = q.shape
    L = S // dilation
    nb = -(-L // half)
    Lp = nb * half
    pad = Lp - L

    def to_sub(t):
        return t.reshape(B, L, dilation, H, Dh).transpose(0, 2, 1, 3, 4)

    qs = jnp.pad(to_sub(q), ((0, 0), (0, 0), (0, pad), (0, 0), (0, 0)))
    kpad = ((0, 0), (0, 0), (half, pad + half), (0, 0), (0, 0))
    ks = jnp.pad(to_sub(k), kpad)
    vs = jnp.pad(to_sub(v), kpad)
    valid = jnp.pad(jnp.ones((L,), dtype=bool), (half, pad + half))

    def bands(t):
        parts = [t[:, :, j * half:j * half + Lp].reshape(B, dilation, nb, half, H, Dh) for j in range(3)]
        return jnp.concatenate(parts, axis=3)

    kb, vb = bands(ks), bands(vs)
    kvalid = jnp.concatenate([valid[j * half:j * half + Lp].reshape(nb, half) for j in range(3)], axis=1)
    qb = qs.reshape(B, dilation, nb, half, H, Dh)
    s = jnp.einsum('brnqhd,brnkhd->brnhqk', qb, kb).astype(jnp.float32) * (Dh ** -0.5)
    rel = jnp.arange(3 * half)[None, :] - half - jnp.arange(half)[:, None]
    mask = (jnp.abs(rel) <= half)[None, :, :] & kvalid[:, None, :]
    s = jnp.where(mask[None, None, :, None, :, :], s, NEG_BIG)
    m = jnp.max(s, axis=-1, keepdims=True)
    e = jnp.exp(s - m)
    den = jnp.sum(e, axis=-1)
    p = (e / den[..., None]).astype(v.dtype)
    o = jnp.einsum('brnhqk,brnkhd->brnqhd', p, vb)
    lse = m[..., 0] + jnp.log(den)
    o = o.reshape(B, dilation, Lp, H, Dh)[:, :, :L].transpose(0, 2, 1, 3, 4).reshape(B, S, H, Dh)
    lse = lse.transpose(0, 1, 2, 4, 3).reshape(B, dilation, Lp, H)[:, :, :L]
    lse = lse.transpose(0, 2, 1, 3).reshape(B, S, H)
    return o, lse


def dilated_attention(q, k, v):
    outs, lses = [], []
    for window, dilation in DIL_BRANCHES:
        o, l = dilated_branch(q, k, v, dilation, window // (2 * dilation))
        outs.append(o)
        lses.append(l)
    wts = jax.nn.softmax(jnp.stack(lses, axis=0), axis=0)
    o = jnp.einsum('nbsh,nbshd->bshd', wts, jnp.stack(outs, axis=0).astype(jnp.float32))
    return o.astype(q.dtype)


def expert_choice_ffn(h, w_router, b_router, w_gate, w_up, w_down):
    B, S, D = h.shape
    cap = EC_CAPACITY_FACTOR * S // N_EXPERTS
    logits = jnp.einsum('bsd,de->bse', h, w_router).astype(jnp.float32) + b_router.astype(jnp.float32)
    aff = jax.nn.softmax(logits, axis=-1)
    gate, idx = lax.top_k(jnp.swapaxes(aff, 1, 2), cap)
    xe = jax.vmap(lambda hb, ib: hb[ib])(h, idx)
    a = jnp.einsum('becd,edf->becf', xe, w_gate)
    u = jnp.einsum('becd,edf->becf', xe, w_up)
    ye = jnp.einsum('becf,efd->becd', jax.nn.silu(a) * u, w_down)
    ye = ye * gate[..., None].astype(ye.dtype)
    return jax.vmap(lambda ib, yb: jnp.zeros((S, D), yb.dtype).at[ib.reshape(-1)].add(yb.reshape(-1, D)))(idx, ye)


def setup_inputs(seed: int = 0) -> dict:
    key = jax.random.key(seed)
    ks = jax.random.split(key, 24)
    f32 = jnp.float32
    nrm = lambda k, shape, fan_in: jax.random.normal(k, shape, f32) * (fan_in ** -0.5)
    gain = lambda k, shape: 1.0 + 0.05 * jax.random.normal(k, shape, f32)
    x = jax.random.normal(ks[0], (BATCH, SEQ, D_MODEL), f32)
    c = jax.random.normal(ks[1], (BATCH, D_MODEL), f32)
    offset = jax.random.randint(ks[2], (BATCH, 1), 0, 1024, dtype=jnp.int32)
    positions = offset + jnp.arange(SEQ, dtype=jnp.int32)[None, :]
    return {
        'x': x,
        'c': c,
        'positions': positions,
        'w_ada': 0.5 * nrm(ks[3], (DEPTH, D_MODEL, N_MOD * D_MODEL), D_MODEL),
        'b_ada': 0.02 * jax.random.normal(ks[4], (DEPTH, N_MOD * D_MODEL), f32),
        'g_mix': gain(ks[5], (DEPTH, D_MODEL)),
        'w_in': nrm(ks[6], (DEPTH, D_MODEL, IN_COLS), D_MODEL),
        'g_q': gain(ks[7], (DEPTH, MLA_Q_RANK)),
        'w_uq': nrm(ks[8], (DEPTH, MLA_Q_RANK, MLA_HEADS * (MLA_NOPE_DIM + MLA_ROPE_DIM)), MLA_Q_RANK),
        'g_kv': gain(ks[9], (DEPTH, MLA_KV_RANK)),
        'w_ukv': nrm(ks[10], (DEPTH, MLA_KV_RANK, MLA_HEADS * (MLA_NOPE_DIM + MLA_V_DIM)), MLA_KV_RANK),
        'w_out': nrm(ks[11], (DEPTH, MIX_WIDTH, D_MODEL), MIX_WIDTH),
        'g_ffn': gain(ks[12], (DEPTH, D_MODEL)),
        'w_router': nrm(ks[13], (DEPTH, D_MODEL, N_EXPERTS), D_MODEL),
        'b_router': 0.01 * jax.random.normal(ks[14], (DEPTH, N_EXPERTS), f32),
        'w_gate': nrm(ks[15], (DEPTH, N_EXPERTS, D_MODEL, EXPERT_FF), D_MODEL),
        'w_up': nrm(ks[16], (DEPTH, N_EXPERTS, D_MODEL, EXPERT_FF), D_MODEL),
        'w_down': nrm(ks[17], (DEPTH, N_EXPERTS, EXPERT_FF, D_MODEL), EXPERT_FF),
        'g_final': gain(ks[18], (D_MODEL,)),
    }


def reference(x, c, positions, w_ada, b_ada, g_mix, w_in, g_q, w_uq, g_kv, w_ukv, w_out,
              g_ffn, w_router, b_router, w_gate, w_up, w_down, g_final):
    B, S, D = x.shape
    cos_m, sin_m = rope_cos_sin(positions, MLA_ROPE_DIM)
    cos_d, sin_d = rope_cos_sin(positions, DIL_ROT_DIM)
    o1 = MLA_Q_RANK
    o2 = o1 + MLA_KV_RANK
    o3 = o2 + MLA_ROPE_DIM
    o4 = o3 + DIL_WIDTH
    o5 = o4 + DIL_WIDTH
    for l in range(DEPTH):
        mod = jnp.einsum('bd,de->be', jax.nn.silu(c), w_ada[l]) + b_ada[l]
        sh_a, sc_a, gt_a, sh_m, sc_m, gt_m = jnp.split(mod[:, None, :], N_MOD, axis=-1)

        h = rms_norm(x, g_mix[l]) * (1.0 + sc_a) + sh_a
        proj = jnp.einsum('bsd,dc->bsc', h, w_in[l])
        cq, ckv, kr = proj[..., :o1], proj[..., o1:o2], proj[..., o2:o3]
        dq, dk, dv = proj[..., o3:o4], proj[..., o4:o5], proj[..., o5:]

        q = jnp.einsum('bsr,rc->bsc', rms_norm(cq, g_q[l]), w_uq[l]).reshape(B, S, MLA_HEADS, MLA_NOPE_DIM + MLA_ROPE_DIM)
        q = jnp.concatenate([q[..., :MLA_NOPE_DIM], apply_rope(q[..., MLA_NOPE_DIM:], cos_m, sin_m)], axis=-1)
        kv = jnp.einsum('bsr,rc->bsc', rms_norm(ckv, g_kv[l]), w_ukv[l]).reshape(B, S, MLA_HEADS, MLA_NOPE_DIM + MLA_V_DIM)
        k_rope = apply_rope(kr[:, :, None, :], cos_m, sin_m)
        k = jnp.concatenate([kv[..., :MLA_NOPE_DIM], jnp.broadcast_to(k_rope, (B, S, MLA_HEADS, MLA_ROPE_DIM))], axis=-1)
        v = kv[..., MLA_NOPE_DIM:]
        out_a = mla_attention(q, k, v).reshape(B, S, MLA_WIDTH)

        def partial_rope(t):
            t = t.reshape(B, S, DIL_HEADS, DIL_HEAD_DIM)
            return jnp.concatenate([apply_rope(t[..., :DIL_ROT_DIM], cos_d, sin_d), t[..., DIL_ROT_DIM:]], axis=-1)
        out_b = dilated_attention(partial_rope(dq), partial_rope(dk), dv.reshape(B, S, DIL_HEADS, DIL_HEAD_DIM))
        out_b = out_b.reshape(B, S, DIL_WIDTH)

        mixed = jnp.einsum('bsc,cd->bsd', jnp.concatenate([out_a, out_b], axis=-1), w_out[l])
        x = x + gt_a * mixed

        h2 = rms_norm(x, g_ffn[l]) * (1.0 + sc_m) + sh_m
        x = x + gt_m * expert_choice_ffn(h2, w_router[l], b_router[l], w_gate[l], w_up[l], w_down[l])
    return rms_norm(x, g_final)
```

```python
import contextlib
import os
import numpy as np
import ml_dtypes
import concourse.bass as bass
import concourse.mybir as mybir
from concourse.bass_utils import run_bass_kernel_spmd

F32 = mybir.dt.float32
BF16 = mybir.dt.bfloat16
I32 = mybir.dt.int32
ALU = mybir.AluOpType
AF = mybir.ActivationFunctionType

S = 8192
D = 1024
NG = S // 512
NW = 8
NO = 4
NCORES = 8
OWN = 2048
CAP = 384
NE = 16
EPS = 1e-6
TWO_PI = 6.283185307179586
DK, DKS, DV, KR, KRS, CQ, CKV, DQ, DQS, WI_COLS = 0, 512, 1024, 1536, 1632, 1728, 2112, 2368, 2880, 3392
N_BISECT = 25


class Buf:
    __slots__ = ("w", "r")

    def __init__(self):
        self.w = None
        self.r = []


class Sched:
    def __init__(self, nc, es):
        self.nc = nc
        self.eng = {"pe": nc.tensor, "act": nc.scalar, "dve": nc.vector, "pool": nc.gpsimd, "sp": nc.sync}
        self.csem = {e: es.enter_context(nc.semaphore("c_" + e)) for e in ("pe", "act", "dve", "pool")}
        self.ccnt = {e: 0 for e in self.csem}
        self.dsem = {"sp": [es.enter_context(nc.semaphore(f"dsp{i}")) for i in range(8)],
                     "pool": [es.enter_context(nc.semaphore(f"dpl{i}")) for i in range(6)],
                     "pre": [es.enter_context(nc.semaphore(f"dpre{i}")) for i in range(4)]}
        self.dcnt = {}
        self.dnext = {"sp": 0, "pool": 0, "pre": 0}
        self.seen = {e: {} for e in self.eng}
        self.sems = {}

    def _wait(self, e, tok):
        sem, val = tok
        key = id(sem)
        if self.seen[e].get(key, 0) >= val:
            return
        self.eng[e].wait_ge(sem, val)
        self.seen[e][key] = val

    def _deps(self, e, reads, writes):
        toks = []
        for b in reads:
            if b.w is not None:
                toks.append(b.w)
        for b in writes:
            if b.w is not None:
                toks.append(b.w)
            toks.extend(b.r)
        for t in toks:
            if e == "pe" and t[0] is self.csem["pe"]:
                continue
            self._wait(e, t)

    def _commit(self, tok, reads, writes):
        for b in reads:
            b.r.append(tok)
        for b in writes:
            b.w = tok
            b.r = []

    def op(self, e, fn, reads=(), writes=()):
        self._deps(e, reads, writes)
        ins = fn(self.eng[e])
        self.ccnt[e] += 1
        sem = self.csem[e]
        ins.then_inc(sem, 1)
        tok = (sem, self.ccnt[e])
        self.sems[id(sem)] = tok
        self._commit(tok, reads, writes)

    def dma(self, q, fn, reads=(), writes=(), ring=None):
        rname = ring or q
        ring = self.dsem[rname]
        sem = ring[self.dnext[rname] % len(ring)]
        self.dnext[rname] += 1
        cnt = self.dcnt.get(id(sem), 0)
        if cnt:
            self._wait(q, (sem, 16 * cnt))
        self._deps(q, reads, writes)
        try:
            ins = fn(self.eng[q])
        except ValueError as ex:
            print("DMA FAIL", ex, "q", q, "dnext", self.dnext, flush=True)
            for trial in range(3):
                try:
                    ins = fn(self.eng[q]); print(" retry ok", trial, flush=True); break
                except ValueError as ex2:
                    print(" retry fail", trial, ex2, flush=True)
            else:
                raise
        ins.then_inc(sem, 16)
        self.dcnt[id(sem)] = cnt + 1
        tok = (sem, 16 * (cnt + 1))
        self.sems[id(sem)] = tok
        self._commit(tok, reads, writes)

    def dma_own(self, es, q, fn, reads=(), writes=()):
        self.nown = getattr(self, "nown", 0) + 1
        sem = es.enter_context(self.nc.semaphore(f"down{self.nown}"))
        self._deps(q, reads, writes)
        ins = fn(self.eng[q])
        ins.then_inc(sem, 16)
        tok = (sem, 16)
        self.csems_coll = getattr(self, "csems_coll", [])
        self.csems_coll.append(tok)
        self._commit(tok, reads, writes)

    def coll(self, es, fn, reads=(), writes=()):
        self.ncoll = getattr(self, "ncoll", 0) + 1
        sem = es.enter_context(self.nc.semaphore(f"ccoll{self.ncoll}"))
        self._deps("pool", reads, writes)
        ins = fn(self.eng["pool"])
        ins.then_inc(sem)
        tok = (sem, 1)
        self.csems_coll = getattr(self, "csems_coll", [])
        self.csems_coll.append(tok)
        self._commit(tok, reads, writes)

    def barrier(self, final=False):
        toks = list(self.sems.values())
        if final:
            toks += getattr(self, "csems_coll", [])
        for e in self.eng:
            for t in toks:
                self._wait(e, t)


def build_program():
    nc = bass.Bass("TRN2", target_bir_lowering=False)
    es = contextlib.ExitStack()

    def din(name, shape, dt=F32):
        return nc.dram_tensor(name, list(shape), dt, kind="ExternalInput")

    dbg = bool(os.environ.get("KDEBUG"))

    def dscr(name, shape, dt):
        if dbg:
            return nc.dram_tensor(name, list(shape), dt, kind="ExternalOutput")
        return nc.dram_tensor(name, list(shape), dt)

    x_d = din("x_win", [4096, D])
    ccol_d = din("c_col", [128, 8])
    pos_d = din("pos_win", [4096], I32)
    valid_d = din("valid_col", [128, 32])
    wada_d = din("w_ada_q", [D, 1536])
    bada_d = din("b_ada_colq", [128, 12])
    gmix_d = din("g_mix_col", [128, 8])
    gffn_d = din("g_ffn_col", [128, 8])
    gfin_d = din("g_final", [D])
    win_d = din("w_in_l", [D, WI_COLS])
    gq_d = din("g_q_col", [128, 3])
    gkv_d = din("g_kv_col", [128, 2])
    wuq_d = din("w_uq_l", [384, 1536])
    wukvk_d = din("w_ukv_k", [256, 512])
    wukvv_d = din("w_ukv_v", [256, 512])
    wout_d = din("w_out", [D, D])
    wr_d = din("w_router", [D, NE])
    br_d = din("b_router", [NE])
    wg_d = din("w_gate", [NE, D, D])
    wu_d = din("w_up", [NE, D, D])
    wd_d = din("w_down", [NE, D, D])
    identf_d = din("ident_f", [128, 128])
    tri_d = din("tri", [128, 128])
    sel_d = din("sel", [128, 64])
    iota_d = din("iota_s", [128, CAP])
    slotc_d = din("slot_col", [128, 3])
    masks_d = din("masks", [128, 20, 512], BF16)
    ropec_d = din("rope_cols", [128, 8])
    y_d = nc.dram_tensor("y", [OWN, D], F32, kind="ExternalOutput")

    qT_all = dscr("qT_own", [8, 96, OWN], BF16)
    kT_send = [dscr(f"kT_send{c}", [192, OWN], BF16) for c in range(4)]
    kT_g = [dscr(f"kT_g{c}", [4 * 192, OWN], BF16) for c in range(4)]
    vaug_send = [dscr(f"vaug_send{c}", [512, 520], BF16) for c in range(4)]
    vaug_all = [dscr(f"vaug_g{c}", [2048, 520], BF16) for c in range(4)]
    dqT_all = dscr("dqT_own", [4, 128, OWN], BF16)
    dkT_all = dscr("dkT_win", [4, 128, 4096], BF16)
    dvaug_all = dscr("dvaug_win", [4096, 520], BF16)
    mixT_all = dscr("mixT_own", [D, OWN], BF16)
    h2_all = dscr("h2_own", [OWN, D], BF16)
    aff_send = dscr("aff_send", [OWN, NE], F32)
    aff_all = dscr("aff_g", [S, NE], F32)
    yacc = dscr("yacc", [OWN + CAP, D], F32)
    mod_send = dscr("mod_send", [128, 12], F32)
    mod_g = dscr("mod_g", [512, 12], F32)
    w16 = {m: dscr(f"w16{m}", [NE, D, D], BF16) for m in "gud"}

    def sb(stack, name, shape, dt=F32):
        return stack.enter_context(nc.sbuf_tensor(name, list(shape), dt))

    with es:
        sc = Sched(nc, es)
        ps = [es.enter_context(nc.psum_tensor(f"ps{i}", [128, 512], F32)) for i in range(6)]
        psb = [Buf() for _ in range(7)]
        pstb = Buf()


        identf = sb(es, "identf", [128, 128]); identb = sb(es, "identb", [128, 128], BF16)
        onesb = sb(es, "onesb", [128, 128], BF16); onesf = sb(es, "onesf", [128, 128])
        ropec = sb(es, "ropec", [128, 8])
        modc = sb(es, "modc", [128, 6, 8])
        g1c = sb(es, "g1c", [128, 8])
        gtab = sb(es, "gtab", [128, 1024]); g2b = sb(es, "g2b", [128, 1024])
        shmb = sb(es, "shmb", [128, 1024]); gtmb = sb(es, "gtmb", [128, 1024])
        gfinb = sb(es, "gfinb", [128, 1024])
        validc = sb(es, "validc", [128, 32]); affo = sb(es, "affo", [128, 16, NE]); affob = Buf()
        kgb = Buf(); vgb = Buf(); affgb = Buf(); affsb = Buf()
        preb = {(m, ex): Buf() for m in "gud" for ex in range(NE)}
        g2c = sb(es, "g2c", [128, 8]); bsrc = sb(es, "bsrc", [128, 128]); bsb = Buf()
        cb = Buf()

        def load(q, dst, src, bufs_w, bufs_r=()):
            sc.dma(q, lambda e: e.dma_start(out=dst, in_=src), reads=bufs_r, writes=bufs_w)

        load("sp", identf[:], identf_d[:, :], [cb])
        load("sp", ropec[:], ropec_d[:, :], [cb])
        load("sp", validc[:], valid_d[:, :], [cb])
        load("sp", gfinb[:], gfin_d.ap().partition_broadcast(128), [cb])
        sc.op("dve", lambda e: e.tensor_copy(out=identb[:], in_=identf[:]), [cb], [cb])
        sc.op("dve", lambda e: e.memset(onesb[:], 1.0), [], [cb])
        sc.op("dve", lambda e: e.memset(onesf[:], 1.0), [], [cb])

        pw = contextlib.ExitStack()
        wi = sb(pw, "wi", [128, 8, WI_COLS], BF16)
        wuq = sb(pw, "wuq", [128, 3, 1536], BF16)
        wkk = sb(pw, "wkk", [128, 2, 512], BF16); wkv = sb(pw, "wkv", [128, 2, 512], BF16)
        gqc = sb(pw, "gqc", [128, 3]); gkvc = sb(pw, "gkvc", [128, 2])
        wb = Buf()
        wba = Buf()

        def load_proj_weights():
            for c0 in range(0, WI_COLS, 848):
                load("pool", wi[:, :, c0:c0 + 848], win_d.ap()[:, c0:c0 + 848].rearrange("(k p) n -> p k n", p=128), [wba if c0 < 1696 else wb])
            load("pool", wuq[:], wuq_d.ap().rearrange("(k p) n -> p k n", p=128), [wb])
            load("pool", wkk[:], wukvk_d.ap().rearrange("(k p) n -> p k n", p=128), [wb])
            load("pool", wkv[:], wukvv_d.ap().rearrange("(k p) n -> p k n", p=128), [wb])
            load("sp", gqc[:], gq_d[:, :], [wb]); load("sp", gkvc[:], gkv_d[:, :], [wb])

        with contextlib.ExitStack() as p0:
            ccol = sb(p0, "ccol", [128, 8]); scol = sb(p0, "scol", [128, 8])
            badac = sb(p0, "badac", [128, 12]); gmixc = sb(p0, "gmixc", [128, 8]); gffnc = sb(p0, "gffnc", [128, 8])
            was = [sb(p0, f"wa{i}", [128, 8, 768], BF16) for i in range(2)]; wabs = [Buf(), Buf()]
            scolb = sb(p0, "scolb", [128, 8], BF16)
            tmpc = sb(p0, "tmpc", [128, 8])
            modp = sb(p0, "modp", [128, 12]); modpb = Buf(); msb = Buf(); mgb_ = Buf()
            load("sp", ccol[:], ccol_d[:, :], [cb]); load("sp", badac[:], bada_d[:, :], [cb])
            load("sp", gmixc[:], gmix_d[:, :], [cb]); load("sp", gffnc[:], gffn_d[:, :], [cb])
            sc.op("act", lambda e: e.activation(out=scol[:], in_=ccol[:], func=AF.Silu), [cb], [cb])
            sc.op("dve", lambda e: e.tensor_copy(out=scolb[:], in_=scol[:]), [cb], [cb])
            for hf in range(2):
                load("pool", was[hf][:], wada_d.ap()[:, hf * 768:(hf + 1) * 768].rearrange("(k p) n -> p k n", p=128), [wabs[hf]])
            load_proj_weights()
            for hf in range(2):
                wa, wab = was[hf], wabs[hf]
                for cc in range(6):
                    for k in range(8):
                        sc.op("pe", lambda e, cc=cc, k=k, wa=wa, hf=hf: e.matmul(ps[5][:, hf * 6 + cc:hf * 6 + cc + 1], lhsT=wa[:, k, cc * 128:(cc + 1) * 128],
                                                                                rhs=scolb[:, k:k + 1], start=(k == 0), stop=(k == 7)),
                              [wab, cb], [psb[5]])
            sc.op("dve", lambda e: e.tensor_tensor(out=modp[:], in0=ps[5][:, 0:12], in1=badac[:], op=ALU.add), [psb[5], cb], [modpb])
            sc.dma("sp", lambda e: e.dma_start(out=mod_send[:, :], in_=modp[:]), [modpb], [msb])
            sc.coll(es, lambda e: e.collective_compute("AllGather", ALU.bypass, replica_groups=[[0, 1, 2, 3], [4, 5, 6, 7]],
                                                       ins=[mod_send.ap().opt()], outs=[mod_g.ap().opt()]), [msb], [mgb_])
            sc.dma("sp", lambda e: e.dma_start(out=modc[:].rearrange("p m k -> p (m k)").rearrange("p (r j) -> p r j", r=4),
                                               in_=mod_g.ap().rearrange("(r p) j -> p r j", p=128)), [mgb_], [cb])
            sc.op("dve", lambda e: e.tensor_scalar(out=tmpc[:], in0=modc[:, 1, :], scalar1=1.0, scalar2=None, op0=ALU.add), [cb], [cb])
            sc.op("dve", lambda e: e.tensor_tensor(out=g1c[:], in0=tmpc[:], in1=gmixc[:], op=ALU.mult), [cb], [cb])
            sc.op("dve", lambda e: e.tensor_scalar(out=tmpc[:], in0=modc[:, 4, :], scalar1=1.0, scalar2=None, op0=ALU.add), [cb], [cb])
            sc.op("dve", lambda e: e.tensor_tensor(out=g2c[:], in0=tmpc[:], in1=gffnc[:], op=ALU.mult), [cb], [cb])
        sc.barrier()

        with contextlib.ExitStack() as p1:

            xss = [sb(p1, f"xs{i}", [128, 4, 1024], BF16) for i in range(2)]; xsb = [Buf(), Buf()]
            xgs = [sb(p1, "xg0", [128, 4, 1024])] * 2; xgbs = [Buf()] * 2
            xjunk = sb(p1, "xjunk", [128, 1024], BF16); xjb = Buf()
            ssc = [sb(p1, f"ssc{i}", [128, 12]) for i in range(2)]; sscb = [Buf(), Buf()]
            sq = sb(p1, "sq", [128, 3, 512], BF16); sqb = Buf()
            hT = sb(p1, "hT", [128, 8, 512], BF16); hTb = Buf()
            rstd = sb(p1, "rstd", [128, 512]); rstdb = Buf()
            posi = sb(p1, "posi", [128, 512], I32); posf = sb(p1, "posf", [128, 512]); posb = Buf()
            ang = sb(p1, "ang", [128, 512]); tq = sb(p1, "tq", [128, 512]); ti = sb(p1, "ti", [128, 512], I32); angb = Buf()
            tabsets = [[sb(p1, f"tab{j}_{i}", [128, 512]) for i in range(4)] for j in range(2)]; tabbsets = [[Buf() for _ in range(4)] for _ in range(2)]
            cT = sb(p1, "cT", [128, 3, 512]); cTb = Buf()
            cn = sb(p1, "cn", [128, 3, 512], BF16); cnb = Buf()
            kvn = sb(p1, "kvn", [128, 2, 512], BF16); kvnb = Buf()
            t1s = [sb(p1, f"t1_{i}", [128, 512]) for i in range(2)]; t2s = [sb(p1, f"t2_{i}", [128, 512]) for i in range(2)]
            t1bs = [Buf() for _ in range(2)]; t2bs = [Buf() for _ in range(2)]; rcn = [0]
            pstbs = [Buf(), Buf()]
            psts = [p1.enter_context(nc.psum_tensor(f"pstA{i}", [128, 1024], BF16)) for i in range(2)]
            qsbs = [sb(p1, f"qsb{i}", [96, 512], BF16) for i in range(2)]; qsbbs = [Buf(), Buf()]
            ksbs = [sb(p1, f"ksb{i}", [96, 512], BF16) for i in range(2)]; ksbbs = [Buf(), Buf()]
            krs = sb(p1, "krs", [96, 512], BF16); krsb = Buf()
            dqs = sb(p1, "dqs", [128, 4, 512], BF16); dqsb = Buf()
            dks = sb(p1, "dks", [128, 4, 512], BF16); dksb = Buf()
            va = sb(p1, "va", [128, 8, 65], BF16); vab = Buf()
            dva = sb(p1, "dva", [128, 8, 65], BF16); dvab = Buf()
            ksendb = Buf(); vsendb = Buf()
            sc.op("pool", lambda e: e.memset(va[:], 1.0), [], [vab])
            sc.op("pool", lambda e: e.memset(dva[:], 1.0), [], [dvab])

            def rsqrt_from(psrc, dst, dstb, scale, rd):
                sc.op("dve", lambda e: e.tensor_scalar(out=dst, in0=psrc, scalar1=scale, scalar2=EPS, op0=ALU.mult, op1=ALU.add), rd, [dstb])
                sc.op("act", lambda e: e.activation(out=dst, in_=dst, func=AF.Sqrt), [dstb], [dstb])
                sc.op("dve", lambda e: e.reciprocal(out=dst, in_=dst), [dstb], [dstb])

            def make_table(si, i, invc, phc):
                sc.op("dve", lambda e: e.tensor_scalar(out=ang[:], in0=posf[:], scalar1=ropec[:, invc:invc + 1], scalar2=ropec[:, phc:phc + 1],
                                                      op0=ALU.mult, op1=ALU.add), [posb, cb], [angb])
                sc.op("dve", lambda e: e.tensor_scalar(out=tq[:], in0=ang[:], scalar1=1.0 / TWO_PI, scalar2=None, op0=ALU.mult), [angb], [angb])
                sc.op("dve", lambda e: e.tensor_copy(out=ti[:], in_=tq[:]), [angb], [angb])
                sc.op("dve", lambda e: e.tensor_copy(out=tq[:], in_=ti[:]), [angb], [angb])
                sc.op("dve", lambda e: e.scalar_tensor_tensor(out=ang[:], in0=tq[:], scalar=-TWO_PI, in1=ang[:], op0=ALU.mult, op1=ALU.add), [angb], [angb])
                sc.op("dve", lambda e: e.tensor_scalar(out=ang[:], in0=ang[:], scalar1=-3.14159, scalar2=3.14159, op0=ALU.max, op1=ALU.min), [angb], [angb])
                sc.op("act", lambda e: e.activation(out=tabsets[si][i][:], in_=ang[:], func=AF.Sin), [angb], [tabbsets[si][i]])

            def fm_chain(pi, col0, M):
                wdeps = [wba] if col0 + M <= 1696 else [wba, wb]
                for k in range(8):
                    sc.op("pe", lambda e, k=k: e.matmul(ps[pi][0:M, :], lhsT=wi[:, k, col0:col0 + M], rhs=hT[:, k, :], start=(k == 0), stop=(k == 7)),
                          wdeps + [hTb], [psb[pi]])

            dvas = [dva, sb(p1, "dva2", [128, 8, 65], BF16)]; dvabs = [dvab, Buf()]
            rr = [0]

            def bank():
                rr[0] = (rr[0] + 1) % 6
                return rr[0]

            def fm_chain(pi, col0, M):
                wdeps = [wba] if col0 + M <= 1696 else [wba, wb]
                for k in range(8):
                    sc.op("pe", lambda e, k=k: e.matmul(ps[pi][0:M, :], lhsT=wi[:, k, col0:col0 + M], rhs=hT[:, k, :], start=(k == 0), stop=(k == 7)),
                          wdeps + [hTb], [psb[pi]])

            def fe_a(wg):
                tok0 = wg * 512
                own = 2 <= wg < 6
                gi = wg % 2
                xg, xgb, xs, xsbuf, ss, ssb = xgs[gi], xgbs[gi], xss[gi], xsb[gi], ssc[gi], sscb[gi]
                load("sp", xg[:], x_d.ap()[tok0:tok0 + 512, :].rearrange("(t p) d -> p t d", p=128), [xgb])
                load("sp", posi[:], pos_d.ap()[tok0:tok0 + 512].partition_broadcast(128), [posb])
                sc.op("dve", lambda e: e.tensor_copy(out=posf[:], in_=posi[:]), [posb], [posb])
                if own:
                    make_table(gi, 0, 0, 1); make_table(gi, 1, 2, 3)
                make_table(gi, 2, 4, 5); make_table(gi, 3, 6, 7)
                for t in range(4):
                    sc.op("act", lambda e, t=t, xg=xg, ss=ss: e.activation(out=xjunk[:], in_=xg[:, t, :], func=AF.Square, accum_out=ss[:, t:t + 1]), [xgb], [xjb, ssb])
                sc.op("dve", lambda e, ss=ss: e.tensor_scalar(out=ss[:, 4:8], in0=ss[:, 0:4], scalar1=1.0 / D, scalar2=EPS, op0=ALU.mult, op1=ALU.add), [ssb], [ssb])
                sc.op("act", lambda e, ss=ss: e.activation(out=ss[:, 4:8], in_=ss[:, 4:8], func=AF.Sqrt), [ssb], [ssb])
                sc.op("dve", lambda e, ss=ss: e.reciprocal(out=ss[:, 8:12], in_=ss[:, 4:8]), [ssb], [ssb])
                for t in range(4):
                    sc.op("dve", lambda e, t=t, xg=xg, xs=xs, ss=ss: e.tensor_scalar(out=xs[:, t, :], in0=xg[:, t, :], scalar1=ss[:, 8 + t:9 + t], scalar2=None, op0=ALU.mult), [xgb, ssb], [xsbuf])

            def fe_b(wg):
                gi = wg % 2
                xs, xsbuf = xss[gi], xsb[gi]
                for k in range(8):
                    hb = k % 2
                    for t in range(4):
                        sc.op("pe", lambda e, k=k, t=t, hb=hb, xs=xs: e.transpose(out=psts[hb][:, t * 128:(t + 1) * 128], in_=xs[:, t, k * 128:(k + 1) * 128], identity=identb[:]),
                              [xsbuf, cb], [pstbs[hb]])
                    sc.op("act", lambda e, k=k, hb=hb: e.activation(out=hT[:, k, :], in_=psts[hb][:, 0:512], func=AF.Identity, scale=g1c[:, k:k + 1], bias=modc[:, 0, k:k + 1]),
                          [pstbs[hb], cb], [hTb])

            def projections(wg):
                tok0 = wg * 512
                own = 2 <= wg < 6
                to = (wg - 2) * 512
                tabs, tabb = tabsets[wg % 2], tabbsets[wg % 2]

                def rope_combine(pa, pb_, lo, hi, tc, ts, dst, dstb):
                    ri = rcn[0] % 2; rcn[0] += 1
                    t1, t2, t1b, t2b = t1s[ri], t2s[ri], t1bs[ri], t2bs[ri]
                    sc.op("dve", lambda e: e.tensor_tensor(out=t1[lo:hi, :], in0=ps[pa][lo:hi, :], in1=tabs[tc][lo:hi, :], op=ALU.mult), [psb[pa], tabb[tc]], [t1b])
                    sc.op("dve", lambda e: e.tensor_tensor(out=t2[lo:hi, :], in0=ps[pb_][lo:hi, :], in1=tabs[ts][lo:hi, :], op=ALU.mult), [psb[pb_], tabb[ts]], [t2b])
                    sc.op("pool", lambda e: e.tensor_tensor(out=dst, in0=t1[lo:hi, :], in1=t2[lo:hi, :], op=ALU.add), [t1b, t2b], [dstb])

                if own:
                    for c in range(3):
                        bk = bank()
                        fm_chain(bk, CQ + c * 128, 128)
                        sc.op("act", lambda e, c=c, bk=bk: e.activation(out=cT[:, c, :], in_=ps[bk][:, :], func=AF.Copy), [psb[bk]], [cTb])
                        sc.op("act", lambda e, c=c, bk=bk: e.activation(out=sq[:, c, :], in_=ps[bk][:, :], func=AF.Square), [psb[bk]], [sqb])
                    bk = bank()
                    for c in range(3):
                        sc.op("pe", lambda e, c=c, bk=bk: e.matmul(ps[bk][:, :], lhsT=onesb[:], rhs=sq[:, c, :], start=(c == 0), stop=(c == 2)), [sqb, cb], [psb[bk]])
                    rsqrt_from(ps[bk][:, :], rstd[:], rstdb, 1.0 / 384, [psb[bk]])
                    for c in range(3):
                        sc.op("dve", lambda e, c=c: e.scalar_tensor_tensor(out=cn[:, c, :], in0=cT[:, c, :], scalar=gqc[:, c:c + 1], in1=rstd[:], op0=ALU.mult, op1=ALU.mult),
                              [cTb, rstdb, wb], [cnb])
                    for h in range(8):
                        ba, bb = bank(), bank()
                        for var, pi in ((0, ba), (768, bb)):
                            for c in range(3):
                                sc.op("pe", lambda e, h=h, var=var, pi=pi, c=c: e.matmul(ps[pi][0:96, :], lhsT=wuq[:, c, var + h * 96: var + (h + 1) * 96], rhs=cn[:, c, :],
                                                                                          start=(c == 0), stop=(c == 2)), [wb, cnb], [psb[pi]])
                        rope_combine(ba, bb, 0, 96, 0, 1, qsbs[h % 2][:, :], qsbbs[h % 2])
                        sc.dma("sp", lambda e, to=to, h=h: e.dma_start(out=qT_all[h, :, to:to + 512], in_=qsbs[h % 2][:]), [qsbbs[h % 2]], [])
                    for c in range(2):
                        bk = bank()
                        fm_chain(bk, CKV + c * 128, 128)
                        sc.op("act", lambda e, c=c, bk=bk: e.activation(out=cT[:, c, :], in_=ps[bk][:, :], func=AF.Copy), [psb[bk]], [cTb])
                        sc.op("act", lambda e, c=c, bk=bk: e.activation(out=sq[:, c, :], in_=ps[bk][:, :], func=AF.Square), [psb[bk]], [sqb])
                    bk = bank()
                    for c in range(2):
                        sc.op("pe", lambda e, c=c, bk=bk: e.matmul(ps[bk][:, :], lhsT=onesb[:], rhs=sq[:, c, :], start=(c == 0), stop=(c == 1)), [sqb, cb], [psb[bk]])
                    rsqrt_from(ps[bk][:, :], rstd[:], rstdb, 1.0 / 256, [psb[bk]])
                    for c in range(2):
                        sc.op("dve", lambda e, c=c: e.scalar_tensor_tensor(out=kvn[:, c, :], in0=cT[:, c, :], scalar=gkvc[:, c:c + 1], in1=rstd[:], op0=ALU.mult, op1=ALU.mult),
                              [cTb, rstdb, wb], [kvnb])
                    ba, bb = bank(), bank()
                    fm_chain(ba, KR, 96); fm_chain(bb, KRS, 96)
                    rope_combine(ba, bb, 64, 96, 0, 1, krs[64:96, :], krsb)
                    for h in range(8):
                        bk = bank()
                        for r in range(2):
                            sc.op("pe", lambda e, h=h, r=r, bk=bk: e.matmul(ps[bk][0:64, :], lhsT=wkk[:, r, h * 64:(h + 1) * 64], rhs=kvn[:, r, :], start=(r == 0), stop=(r == 1)),
                                  [wb, kvnb], [psb[bk]])
                        sc.op("act", lambda e, h=h, bk=bk: e.activation(out=ksbs[h % 2][0:64, :], in_=ps[bk][0:64, :], func=AF.Copy), [psb[bk]], [ksbbs[h % 2]])
                        sc.op("pool", lambda e, h=h: e.tensor_copy(out=ksbs[h % 2][64:96, :], in_=krs[64:96, :]), [krsb], [ksbbs[h % 2]])
                        sc.dma("sp", lambda e, to=to, h=h: e.dma_start(out=kT_send[h // 2][(h % 2) * 96:(h % 2 + 1) * 96, to:to + 512], in_=ksbs[h % 2][:]), [ksbbs[h % 2]], [ksendb])
                    for t in range(4):
                        bk = bank()
                        for r in range(2):
                            sc.op("pe", lambda e, t=t, r=r, bk=bk: e.matmul(ps[bk][:, :], lhsT=kvn[:, r, t * 128:(t + 1) * 128], rhs=wkv[:, r, :], start=(r == 0), stop=(r == 1)),
                                  [wb, kvnb], [psb[bk]])
                        sc.op("act", lambda e, bk=bk: e.activation(out=va[:, :, 0:64], in_=ps[bk][:, :].rearrange("p (h c) -> p h c", h=8), func=AF.Copy), [psb[bk]], [vab])
                        sc.dma("sp", lambda e, t=t, og=wg - 2: e.dma_start(out=vaug_send[og][t * 128:(t + 1) * 128, :], in_=va[:].rearrange("p h c -> p (h c)")), [vab], [vsendb])
                    for pr in range(4):
                        ba, bb = bank(), bank()
                        fm_chain(ba, DQ + pr * 128, 128); fm_chain(bb, DQS + pr * 128, 128)
                        rope_combine(ba, bb, 0, 128, 2, 3, dqs[:, pr, :], dqsb)
                    sc.dma("sp", lambda e, to=to: e.dma_start(out=dqT_all.ap()[:, :, to:to + 512].rearrange("h r t -> r h t"), in_=dqs[:]), [dqsb], [])
                for pr in range(4):
                    ba, bb = bank(), bank()
                    fm_chain(ba, DK + pr * 128, 128); fm_chain(bb, DKS + pr * 128, 128)
                    rope_combine(ba, bb, 0, 128, 2, 3, dks[:, pr, :], dksb)
                sc.dma("sp", lambda e, tok0=tok0: e.dma_start(out=dkT_all.ap()[:, :, tok0:tok0 + 512].rearrange("h r t -> r h t"), in_=dks[:]), [dksb], [])
                for t in range(4):
                    bk = bank()
                    for k in range(8):
                        sc.op("pe", lambda e, t=t, k=k, bk=bk: e.matmul(ps[bk][:, :], lhsT=hT[:, k, t * 128:(t + 1) * 128], rhs=wi[:, k, DV:DV + 512], start=(k == 0), stop=(k == 7)),
                              [wba, hTb], [psb[bk]])
                    vc = wg * 4 + t
                    dv_ = dvas[t % 2]; dvb_ = dvabs[t % 2]
                    sc.op("act", lambda e, bk=bk, vc=vc, dv_=dv_: e.activation(out=dv_[:, :, 0:64], in_=ps[bk][:, :].rearrange("p (h c) -> p h c", h=8), func=AF.Copy, scale=validc[:, vc:vc + 1]),
                          [psb[bk], cb], [dvb_])
                    sc.op("dve", lambda e, vc=vc, dv_=dv_: e.tensor_copy(out=dv_[:, :, 64], in_=validc[:, vc:vc + 1].broadcast_to([128, 8])), [cb], [dvb_])
                    sc.dma("sp", lambda e, t=t, tok0=tok0, dv_=dv_: e.dma_start(out=dvaug_all[tok0 + t * 128: tok0 + (t + 1) * 128, :],
                                                                          in_=dv_[:].rearrange("p h c -> p (h c)")), [dvb_], [])
            sc.op("pe", lambda e: e.matmul(ps[0][:, 0:128], lhsT=onesb[:], rhs=onesb[:, 0:128], start=True, stop=True), [cb], [psb[0]])
            fe_a(0); fe_b(0)
            for wg in range(NW):
                if wg + 1 < NW:
                    fe_a(wg + 1)
                projections(wg)
                if wg + 1 < NW:
                    fe_b(wg + 1)
            for c4 in range(4):
                sc.coll(es, lambda e, c4=c4: e.collective_compute("AllGather", ALU.bypass, replica_groups=[[0, 1, 2, 3], [4, 5, 6, 7]],
                                                                  ins=[kT_send[c4].ap().opt()], outs=[kT_g[c4].ap().opt()]), [ksendb], [kgb])
            for c4 in range(4):
                sc.coll(es, lambda e, c4=c4: e.collective_compute("AllGather", ALU.bypass, replica_groups=[[0, 1, 2, 3], [4, 5, 6, 7]],
                                                                  ins=[vaug_send[c4].ap().opt()], outs=[vaug_all[c4].ap().opt()]), [vsendb], [vgb])
        sc.barrier()
        pw.close()

        if os.environ.get("KSTOP") == "a1":
            sc.barrier(final=True)
            return nc
        pwo = contextlib.ExitStack()
        wo = sb(pwo, "wo", [128, 8, 1024], BF16); wob = Buf()
        with contextlib.ExitStack() as p2:
            kTs = [sb(p2, f"kT{i}", [128, S], BF16) for i in range(2)]; kTbs = [Buf(), Buf()]
            qTs = [sb(p2, f"qT{i}", [128, OWN], BF16) for i in range(2)]; qTbs = [Buf(), Buf()]
            vvs = [sb(p2, f"vv{i}", [128, 64, 65], BF16) for i in range(2)]; vvbs = [Buf(), Buf()]
            msk = sb(p2, "msk", [128, 20, 512], BF16); mskb = Buf()
            selt = sb(p2, "selt", [128, 64]);
            ps.append(p2.enter_context(nc.psum_tensor("ps6a", [128, 512], F32)))
            SB = [0, 1, 2, 6]
            pts = [sb(p2, f"pt{i}", [128, 512], BF16) for i in range(4)]; ptb = [Buf() for _ in range(4)]
            osb = [sb(p2, f"osb{i}", [65, 512]) for i in range(2)]; osbb = [Buf(), Buf()]
            rden = [sb(p2, f"rden{i}", [64, 512]) for i in range(2)]; rdenb = [Buf(), Buf()]
            oT = [sb(p2, f"oT{i}", [64, 512], BF16) for i in range(2)]; oTb = [Buf(), Buf()]
            mskbs = [Buf() for _ in range(4)]
            load("sp", selt[:], sel_d[:, :], [mskb])
            load("pool", wo[:], wout_d.ap().rearrange("(k p) n -> p k n", p=128), [wob])
            blk = 0
            pace = Buf()

            def attend(kT, kTb, qT, qTb, vv, vvb, nkt, kbase_fn, vidx_fn, lo, hi, scale, masked, row0, qb, blk):
                po = 3 + (blk % 2)
                LA = 2
                for kt in range(nkt + LA):
                    if kt < nkt:
                        si = kt % 4
                        kc = kbase_fn(kt)
                        sc.op("pe", lambda e, si=si, kc=kc: e.matmul(ps[SB[si]][:, :], lhsT=kT[lo:hi, kc:kc + 128], rhs=qT[lo:hi, qb * 512:(qb + 1) * 512], start=True, stop=not masked),
                              [kTb, qTb], [psb[SB[si]]])
                        if masked:
                            sc.op("pe", lambda e, si=si, kt=kt: e.matmul(ps[SB[si]][:, :], lhsT=identb[:], rhs=msk[:, kt, :], start=False, stop=True),
                                  [mskbs[kt // 5], cb], [psb[SB[si]]])
                        sc.op("act", lambda e, si=si: e.activation(out=pts[si][:], in_=ps[SB[si]][:, :], func=AF.Exp, scale=scale), [psb[SB[si]]], [ptb[si]])
                    if kt >= LA:
                        k0 = kt - LA
                        si = k0 % 4
                        vt = vidx_fn(k0)
                        sc.op("pe", lambda e, si=si, vt=vt, k0=k0: e.matmul(ps[po][0:65, :], lhsT=vv[:, vt, :], rhs=pts[si][:], start=(k0 == 0), stop=(k0 == nkt - 1)),
                              [vvb, ptb[si]], [psb[po]])
                oi = blk % 2
                sc.op("act", lambda e: e.activation(out=osb[oi][:], in_=ps[po][0:65, :], func=AF.Copy), [psb[po]], [osbb[oi]])
                sc.op("pe", lambda e: e.matmul(ps[5][0:64, :], lhsT=selt[0:65, :], rhs=osb[oi][:], start=True, stop=True), [osbb[oi], mskb], [psb[5]])
                sc.op("dve", lambda e: e.reciprocal(out=rden[oi][:], in_=ps[5][0:64, :]), [psb[5]], [rdenb[oi]])
                sc.op("dve", lambda e: e.tensor_tensor(out=oT[oi][:], in0=osb[oi][0:64, :], in1=rden[oi][:], op=ALU.mult), [osbb[oi], rdenb[oi]], [oTb[oi], pace])
                sc.dma("sp", lambda e: e.dma_start(out=mixT_all[row0:row0 + 64, qb * 512:(qb + 1) * 512], in_=oT[oi][:]), [oTb[oi]], [])

            qdil = [[sb(p2, f"qdil{i}{j}", [128, OWN], BF16) for j in range(2)] for i in range(2)]; qdilb = [Buf(), Buf()]
            for i in range(2):
                sc.op("dve", lambda e, i=i: e.memset(qdil[i][0][64:128, :], 0.0), [], [qdilb[i]])
                sc.op("dve", lambda e, i=i: e.memset(qdil[i][1][0:64, :], 0.0), [], [qdilb[i]])

            def load_kq(kind, idx, bi):
                kT, kTb, qT, qTb = kTs[bi], kTbs[bi], qTs[bi], qTbs[bi]
                if kind == "dil":
                    load("sp", kT[:, 0:4096], dkT_all[idx, :, :], [kTb])
                    load("sp", qdil[bi][0][0:64, :], dqT_all[idx, 0:64, :], [qdilb[bi]])
                    load("sp", qdil[bi][1][64:128, :], dqT_all[idx, 64:128, :], [qdilb[bi]])
                else:
                    h = idx
                    for r in range(4):
                        load("sp", kT[0:96, r * OWN:(r + 1) * OWN], kT_g[h // 2][r * 192 + (h % 2) * 96: r * 192 + (h % 2 + 1) * 96, :], [kTb], [kgb])
                    load("sp", qT[0:96, :], qT_all[h, :, :], [qTb])

            def load_v(kind, h, bi):
                vv, vvb = vvs[bi], vvbs[bi]
                if kind == "dil":
                    for j0 in range(0, 32, 16):
                        load("sp", vv[:, j0:j0 + 16, :], dvaug_all.ap()[j0 * 128:(j0 + 16) * 128, h * 65:(h + 1) * 65].rearrange("(j p) c -> p j c", p=128), [vvb])
                else:
                    for r in range(4):
                        for c4 in range(4):
                            j0 = r * 16 + c4 * 4
                            load("sp", vv[:, j0:j0 + 4, :], vaug_all[c4].ap()[r * 512:(r + 1) * 512, h * 65:(h + 1) * 65].rearrange("(j p) c -> p j c", p=128), [vvb], [vgb])

            jobs = [("dil", h) for h in range(8)] + [("mla", h) for h in range(8)]
            kq_slot = {}
            nkq = 0

            def issue_loads(ji):
                nonlocal_kq = kq_slot
                kind, h = jobs[ji]
                key = (kind, h // 2) if kind == "dil" else (kind, h)
                if key not in nonlocal_kq:
                    nonlocal_kq[key] = len(nonlocal_kq) % 2
                    load_kq(kind, key[1], nonlocal_kq[key])
                load_v(kind, h, ji % 2)

            kq_slot[("dil", 0)] = 0
            load_kq("dil", 0, 0)
            load("sp", msk[:, 0:5, :], masks_d[:, 0:5, :], [mskbs[0]])
            load_v("dil", 0, 0)
            for c5 in range(1, 4):
                load("sp", msk[:, c5 * 5:(c5 + 1) * 5, :], masks_d[:, c5 * 5:(c5 + 1) * 5, :], [mskbs[c5]])
            wsrc = {"g": wg_d, "u": wu_d, "d": wd_d}
            precast = [(m, ex) for ex in range(NE) for m in "gud"]
            pci = [0]
            for ji, (kind, h) in enumerate(jobs):
                if ji + 1 < len(jobs):
                    issue_loads(ji + 1)
                key = (kind, h // 2) if kind == "dil" else (kind, h)
                bi = kq_slot[key]
                for qb in range(NO):
                    if kind == "dil":
                        hh = h % 2
                        attend(kTs[bi], kTbs[bi], qdil[bi][hh], qdilb[bi], vvs[ji % 2], vvbs[ji % 2], 20, lambda kt, qb=qb: qb * 512 + kt * 128, lambda k0, qb=qb: qb * 4 + k0,
                               0, 128, 0.125, True, 512 + h * 64, qb, blk)
                    else:
                        attend(kTs[bi], kTbs[bi], qTs[bi], qTbs[bi], vvs[ji % 2], vvbs[ji % 2], 64, lambda kt: kt * 128, lambda k0: k0, 0, 96, 96 ** -0.5, False, h * 64, qb, blk)
                    blk += 1
                    if blk >= 8 and not (24 <= blk < 32) and pci[0] < len(precast):
                        m, ex = precast[pci[0]]; pci[0] += 1
                        sc.dma("pool", lambda e, m=m, ex=ex: e.dma_start(out=w16[m][ex, :, :], in_=wsrc[m][ex, :, :]), [pace], [preb[(m, ex)]], ring="pre")
            for src_col, dst in ((modc[:, 2, :], gtab), (g2c[:], g2b), (modc[:, 3, :], shmb), (modc[:, 5, :], gtmb)):
                for half in range(2):
                    for kk in range(4):
                        k = half * 4 + kk
                        sc.op("dve", lambda e, k=k, src_col=src_col: e.tensor_scalar(out=bsrc[:], in0=onesf[:], scalar1=src_col[:, k:k + 1],
                                                                                    scalar2=None, op0=ALU.mult), [cb], [bsb])
                        sc.op("pe", lambda e, kk=kk: e.matmul(ps[5][:, kk * 128:(kk + 1) * 128], lhsT=bsrc[:], rhs=identf[:], start=True, stop=True),
                              [bsb, cb], [psb[5]])
                    sc.op("dve", lambda e, half=half, dst=dst: e.tensor_copy(out=dst[:, half * 512:(half + 1) * 512], in_=ps[5][:, :]), [psb[5]], [cb])
            for c in range(8):
                sc.op("dve", lambda e, c=c: e.tensor_tensor(out=wo[:, c, :], in0=wo[:, c, :], in1=gtab[:], op=ALU.mult), [wob, cb], [wob])
            ps.pop()
        sc.barrier()

        yaccb = Buf(); h2ob = Buf()
        with contextlib.ExitStack() as p3:
            wr = sb(p3, "wr", [128, 8, NE]); brb = sb(p3, "brb", [128, NE]); w3b = Buf()
            load("sp", wr[:], wr_d.ap().rearrange("(k p) n -> p k n", p=128), [w3b])
            load("sp", brb[:], br_d.ap().partition_broadcast(128), [w3b])
            mg = sb(p3, "mg", [128, 8, 512], BF16); mgb = Buf()
            xts = [sb(p3, f"xt{i}", [128, 1024]) for i in range(2)]; xtb = [Buf(), Buf()]
            x1s = [sb(p3, f"x1{i}", [128, 1024]) for i in range(2)]; x1b = [Buf(), Buf()]
            h2s = [sb(p3, f"h2{i}", [128, 1024]) for i in range(2)]; h2b = [Buf(), Buf()]
            junk = sb(p3, "junk", [128, 1024]); junkb = Buf()
            h2T = sb(p3, "h2T", [128, 8, 128]); h2Tb = Buf()
            sms = [sb(p3, f"sm{i}", [128, 8]) for i in range(2)]; smb = [Buf(), Buf()]
            lg = sb(p3, "lg", [128, NE]); lgb = Buf()
            mgs = [mg, sb(p3, "mg2", [128, 8, 512], BF16)]; mgbs = [mgb, Buf()]

            def stage_a(tt):
                g, t = tt // 4, tt % 4
                r0 = tt * 128
                i = tt % 2
                xt, x1, h2, sm = xts[i], x1s[i], h2s[i], sms[i]
                mgc, mgcb = mgs[g % 2], mgbs[g % 2]
                if t == 0 and g == 0:
                    load("sp", mgc[:], mixT_all.ap()[:, g * 512:(g + 1) * 512].rearrange("(c p) t -> p c t", p=128), [mgcb])
                if t == 0 and g + 1 < NO:
                    load("sp", mgs[(g + 1) % 2][:], mixT_all.ap()[:, (g + 1) * 512:(g + 2) * 512].rearrange("(c p) t -> p c t", p=128), [mgbs[(g + 1) % 2]])
                load("sp", xt[:], x_d[1024 + r0:1024 + r0 + 128, :], [xtb[i]])
                for dh in range(2):
                    for c in range(8):
                        sc.op("pe", lambda e, dh=dh, c=c, t=t: e.matmul(ps[dh][:, :], lhsT=mgc[:, c, t * 128:(t + 1) * 128], rhs=wo[:, c, dh * 512:(dh + 1) * 512],
                                                                      start=(c == 0), stop=(c == 7)), [mgcb, wob], [psb[dh]])
                    sc.op("dve", lambda e, dh=dh, x1=x1, xt=xt: e.tensor_tensor(out=x1[:, dh * 512:(dh + 1) * 512], in0=ps[dh][:, :], in1=xt[:, dh * 512:(dh + 1) * 512], op=ALU.add),
                          [psb[dh], xtb[i]], [x1b[i]])
                sc.dma("pool", lambda e, r0=r0, x1=x1: e.dma_start(out=yacc[r0:r0 + 128, :], in_=x1[:]), [x1b[i]], [yaccb])
                sc.op("act", lambda e, x1=x1, sm=sm: e.activation(out=junk[:], in_=x1[:], func=AF.Square, accum_out=sm[:, 0:1]), [x1b[i]], [junkb, smb[i]])
                sc.op("dve", lambda e, sm=sm: e.tensor_scalar(out=sm[:, 1:2], in0=sm[:, 0:1], scalar1=1.0 / D, scalar2=EPS, op0=ALU.mult, op1=ALU.add), [smb[i]], [smb[i]])
                sc.op("act", lambda e, sm=sm: e.activation(out=sm[:, 1:2], in_=sm[:, 1:2], func=AF.Sqrt), [smb[i]], [smb[i]])
                sc.op("dve", lambda e, sm=sm: e.reciprocal(out=sm[:, 2:3], in_=sm[:, 1:2]), [smb[i]], [smb[i]])
                sc.op("dve", lambda e, sm=sm, h2=h2, x1=x1: e.scalar_tensor_tensor(out=h2[:], in0=x1[:], scalar=sm[:, 2:3], in1=g2b[:], op0=ALU.mult, op1=ALU.mult), [x1b[i], smb[i], cb], [h2b[i]])
                sc.op("dve", lambda e, h2=h2: e.tensor_tensor(out=h2[:], in0=h2[:], in1=shmb[:], op=ALU.add), [h2b[i], cb], [h2b[i]])
                sc.dma("pool", lambda e, r0=r0, h2=h2: e.dma_start(out=h2_all[r0:r0 + 128, :], in_=h2[:]), [h2b[i]], [h2ob])

            def stage_b(tt):
                r0 = tt * 128
                i = tt % 2
                h2, sm = h2s[i], sms[i]
                for half in range(2):
                    for kk in range(4):
                        k = half * 4 + kk
                        sc.op("pe", lambda e, k=k, kk=kk, half=half, h2=h2: e.transpose(out=ps[2 + half][:, kk * 128:(kk + 1) * 128], in_=h2[:, k * 128:(k + 1) * 128], identity=identf[:]),
                              [h2b[i], cb], [psb[2 + half]])
                    sc.op("act", lambda e, half=half: e.activation(out=h2T[:, half * 4:(half + 1) * 4, :], in_=ps[2 + half][:, :].rearrange("p (k t) -> p k t", k=4), func=AF.Copy),
                          [psb[2 + half]], [h2Tb])
                for k in range(8):
                    sc.op("pe", lambda e, k=k: e.matmul(ps[4][:, 0:NE], lhsT=h2T[:, k, :], rhs=wr[:, k, :], start=(k == 0), stop=(k == 7)), [h2Tb, w3b], [psb[4]])
                sc.op("dve", lambda e: e.tensor_tensor(out=lg[:], in0=ps[4][:, 0:NE], in1=brb[:], op=ALU.add), [psb[4], w3b], [lgb])
                sc.op("dve", lambda e, sm=sm: e.reduce_max(out=sm[:, 3:4], in_=lg[:], axis=mybir.AxisListType.X), [lgb], [smb[i]])
                sc.op("dve", lambda e, sm=sm: e.tensor_scalar(out=sm[:, 4:5], in0=sm[:, 3:4], scalar1=-1.0, scalar2=None, op0=ALU.mult), [smb[i]], [smb[i]])
                sc.op("act", lambda e, sm=sm: e.activation(out=lg[:], in_=lg[:], func=AF.Exp, bias=sm[:, 4:5], scale=1.0, accum_out=sm[:, 5:6]), [lgb, smb[i]], [lgb, smb[i]])
                sc.op("dve", lambda e, sm=sm: e.reciprocal(out=sm[:, 6:7], in_=sm[:, 5:6]), [smb[i]], [smb[i]])
                sc.op("dve", lambda e, sm=sm, tt=tt: e.tensor_scalar(out=affo[:, tt, :], in0=lg[:], scalar1=sm[:, 6:7], scalar2=None, op0=ALU.mult), [lgb, smb[i]], [affob])
                sc.dma("pool", lambda e, r0=r0, tt=tt: e.dma_start(out=aff_send[r0:r0 + 128, :], in_=affo[:, tt, :]), [affob], [affsb])

            stage_a(0)
            for tt in range(16):
                if tt + 1 < 16:
                    stage_a(tt + 1)
                stage_b(tt)
            sc.coll(es, lambda e: e.collective_compute("AllGather", ALU.bypass, replica_groups=[[0, 1, 2, 3], [4, 5, 6, 7]],
                                                       ins=[aff_send.ap().opt()], outs=[aff_all.ap().opt()]), [affsb], [affgb])
        sc.barrier()
        pwo.close()

        with contextlib.ExitStack() as p4:
            wgs = [sb(p4, f"wg{i}", [128, 8, 1024], BF16) for i in range(2)]
            wus = [sb(p4, f"wu{i}", [128, 8, 1024], BF16) for i in range(2)]
            wds = [sb(p4, f"wd{i}", [128, 8, 1024], BF16) for i in range(2)]
            wgb = [Buf(), Buf()]; wub = [Buf(), Buf()]; wdb = [Buf(), Buf()]

            def load_expert(ex_):
                i = ex_ % 2
                q4 = ex_ // 4
                load("sp", wgs[i][:, :, :], w16["g"].ap()[ex_].rearrange("(k p) n -> p k n", p=128), [wgb[i]], [preb[("g", ex_)]])
                load("sp", wus[i][:, :, :], w16["u"].ap()[ex_].rearrange("(k p) n -> p k n", p=128), [wub[i]], [preb[("u", ex_)]])
                load("sp", wds[i][:, :, :], w16["d"].ap()[ex_].rearrange("(k p) n -> p k n", p=128), [wdb[i]], [preb[("d", ex_)]])

            ps.append(p4.enter_context(nc.psum_tensor("ps6b", [128, 512], F32)))
            pst = p4.enter_context(nc.psum_tensor("pstB", [128, 1024], BF16))
            load_expert(0); load_expert(1)
            aff = sb(p4, "aff", [128, 64, NE]); affb = Buf()
            cmp = sb(p4, "cmp", [128, 64, NE]); cmpb = Buf()
            cntp = sb(p4, "cntp", [128, NE]); cntb = Buf()
            lo = sb(p4, "lo", [128, NE]); hi = sb(p4, "hi", [128, NE]); mid = sb(p4, "mid", [128, NE]); ge = sb(p4, "ge", [128, NE]); dlt = sb(p4, "dlt", [128, NE])
            bsb_ = Buf()
            load("sp", aff[:], aff_all.ap().rearrange("(p j) e -> p j e", p=128), [affb], [affgb])
            sc.op("dve", lambda e: e.memset(lo[:], 0.0), [], [bsb_])
            sc.op("dve", lambda e: e.memset(hi[:], 1.0), [], [bsb_])
            sc.op("dve", lambda e: e.memset(mid[:], 0.5), [], [bsb_])
            for it in range(N_BISECT):
                sc.op("dve", lambda e: e.tensor_tensor(out=cmp[:], in0=aff[:], in1=mid[:].rearrange("p (o e) -> p o e", o=1).broadcast_to([128, 64, NE]), op=ALU.is_gt), [affb, bsb_], [cmpb])
                sc.op("dve", lambda e: e.tensor_reduce(out=cntp[:], in_=cmp[:].rearrange("p j e -> p e j"), axis=mybir.AxisListType.X, op=ALU.add), [cmpb], [cntb])
                sc.op("pe", lambda e: e.matmul(ps[0][:, 0:NE], lhsT=onesf[:], rhs=cntp[:], start=True, stop=True), [cntb, cb], [psb[0]])
                sc.op("dve", lambda e: e.tensor_scalar(out=ge[:], in0=ps[0][:, 0:NE], scalar1=1024.0, scalar2=None, op0=ALU.is_ge), [psb[0]], [bsb_])
                sc.op("dve", lambda e: e.tensor_tensor(out=dlt[:], in0=mid[:], in1=lo[:], op=ALU.subtract), [bsb_], [bsb_])
                sc.op("dve", lambda e: e.tensor_tensor(out=dlt[:], in0=dlt[:], in1=ge[:], op=ALU.mult), [bsb_], [bsb_])
                sc.op("dve", lambda e: e.tensor_tensor(out=lo[:], in0=lo[:], in1=dlt[:], op=ALU.add), [bsb_], [bsb_])
                sc.op("dve", lambda e: e.tensor_tensor(out=dlt[:], in0=hi[:], in1=mid[:], op=ALU.subtract), [bsb_], [bsb_])
                sc.op("dve", lambda e: e.tensor_tensor(out=dlt[:], in0=dlt[:], in1=ge[:], op=ALU.mult), [bsb_], [bsb_])
                sc.op("dve", lambda e: e.tensor_tensor(out=hi[:], in0=mid[:], in1=dlt[:], op=ALU.add), [bsb_], [bsb_])
                sc.op("dve", lambda e: e.tensor_tensor(out=mid[:], in0=lo[:], in1=hi[:], op=ALU.add), [bsb_], [bsb_])
                sc.op("dve", lambda e: e.tensor_scalar(out=mid[:], in0=mid[:], scalar1=0.5, scalar2=None, op0=ALU.mult), [bsb_], [bsb_])

            trit = sb(p4, "trit", [128, 128]); trib = sb(p4, "trib", [128, 128], BF16)
            iot = sb(p4, "iot", [128, CAP]); slotc = sb(p4, "slotc", [128, 3]); ob = Buf()
            load("sp", trit[:], tri_d[:, :], [ob]); load("sp", iot[:], iota_d[:, :], [ob]); load("sp", slotc[:], slotc_d[:, :], [ob])
            sc.op("dve", lambda e: e.tensor_copy(out=trib[:], in_=trit[:]), [ob], [ob])
            mk = sb(p4, "mk", [128, 16, NE], BF16); mkb = Buf()
            cc = sb(p4, "cc", [128, 16, NE]); ccb = Buf()
            ex = sb(p4, "ex", [128, 16, NE]); exb = Buf()
            sc.op("dve", lambda e: e.tensor_tensor(out=mk[:], in0=affo[:], in1=lo[:].rearrange("p (o e) -> p o e", o=1).broadcast_to([128, 16, NE]), op=ALU.is_gt), [affob, bsb_], [mkb])
            sc.op("pe", lambda e: e.matmul(ps[0][:, 0:256], lhsT=trib[:], rhs=mk[:].rearrange("p j e -> p (j e)"), start=True, stop=True), [mkb, ob], [psb[0]])
            sc.op("pe", lambda e: e.matmul(ps[1][:, 0:256], lhsT=onesb[:], rhs=mk[:].rearrange("p j e -> p (j e)"), start=True, stop=True), [mkb, cb], [psb[1]])
            sc.op("dve", lambda e: e.tensor_copy(out=cc[:].rearrange("p j e -> p (j e)"), in_=ps[0][:, 0:256]), [psb[0]], [ccb])
            sc.op("dve", lambda e: e.memset(ex[:, 0, :], 0.0), [], [exb])
            for j in range(1, 16):
                sc.op("dve", lambda e, j=j: e.tensor_tensor(out=ex[:, j, :], in0=ex[:, j - 1, :], in1=ps[1][:, (j - 1) * NE:j * NE], op=ALU.add), [exb, psb[1]], [exb])
            sc.op("dve", lambda e: e.tensor_tensor(out=cc[:], in0=cc[:], in1=ex[:], op=ALU.add), [ccb, exb], [ccb])
            lt = [sb(p4, f"lt{i}", [128, CAP], BF16) for i in range(16)]; ltb = [Buf() for _ in range(16)]
            idxf = sb(p4, "idxf", [128, 3, NE]); idxb = Buf()
            gidx = sb(p4, "gidx", [128, 3, NE], I32); sidx = sb(p4, "sidx", [128, 3, NE], I32); tmpi = sb(p4, "tmpi", [128, 3, NE])
            idxbs = [Buf() for _ in range(NE)]
            slot3 = slotc[:].rearrange("p (s o) -> p s o", o=1)

            def idx_dve(ex_):
                for j in range(16):
                    sc.op("dve", lambda e, j=j, ex_=ex_: e.tensor_scalar(out=lt[j][:], in0=iot[:], scalar1=cc[:, j, ex_:ex_ + 1], scalar2=None, op0=ALU.is_ge),
                          [ob, ccb], [ltb[j]])

            def idx_pe(ex_):
                for st in range(3):
                    for j in range(16):
                        sc.op("pe", lambda e, j=j, ex_=ex_, st=st: e.matmul(ps[5][:, st * NE + ex_: st * NE + ex_ + 1], lhsT=lt[j][:, st * 128:(st + 1) * 128],
                                                                           rhs=onesb[:, 0:1], start=(j == 0), stop=(j == 15)), [ltb[j], cb], [psb[5]])
                ib = idxbs[ex_]
                sl = slice(ex_, ex_ + 1)
                sc.op("dve", lambda e: e.tensor_copy(out=idxf[:, :, sl], in_=ps[5][:, 0:3 * NE].rearrange("p (s e) -> p s e", s=3)[:, :, sl]), [psb[5]], [ib])
                sc.op("dve", lambda e: e.tensor_scalar(out=tmpi[:, :, sl], in0=idxf[:, :, sl], scalar1=2047.0, scalar2=None, op0=ALU.min), [ib, ob], [ib])
                sc.op("dve", lambda e: e.tensor_copy(out=gidx[:, :, sl], in_=tmpi[:, :, sl]), [ib], [ib])
                sc.op("dve", lambda e: e.tensor_scalar(out=tmpi[:, :, sl], in0=idxf[:, :, sl], scalar1=2048.0, scalar2=None, op0=ALU.is_ge), [ib], [ib])
                sc.op("dve", lambda e: e.tensor_tensor(out=tmpi[:, :, sl], in0=tmpi[:, :, sl], in1=slot3, op=ALU.mult), [ib, ob], [ib])
                sc.op("dve", lambda e: e.tensor_tensor(out=tmpi[:, :, sl], in0=tmpi[:, :, sl], in1=idxf[:, :, sl], op=ALU.add), [ib], [ib])
                sc.op("dve", lambda e: e.tensor_copy(out=sidx[:, :, sl], in_=tmpi[:, :, sl]), [ib], [ib])

            xe = [sb(p4, f"xe{i}", [128, 1024], BF16) for i in range(6)]; xeb = [Buf() for _ in range(6)]
            gat = [sb(p4, f"gat{i}", [128, NE]) for i in range(6)]; gatb = [Buf() for _ in range(6)]
            xeT = sb(p4, "xeT", [128, 8, CAP], BF16); xeTb = Buf()
            hh_ = sb(p4, "hh", [128, 8, CAP], BF16); hhb = Buf()
            sils = [sb(p4, f"sil{i}", [128, CAP]) for i in range(2)]; silb = [Buf(), Buf()]
            ye = [sb(p4, f"ye{i}", [128, 1024]) for i in range(2)]; yeb = [Buf(), Buf()]

            def gathers(ex_):
                for st in range(3):
                    xi = (ex_ % 2) * 3 + st
                    sc.dma("pool", lambda e, st=st, ex_=ex_, xi=xi: e.indirect_dma_start(out=xe[xi][:, :], out_offset=None, in_=h2_all[:, :],
                                                                                         in_offset=bass.IndirectOffsetOnAxis(ap=gidx[:, st, ex_:ex_ + 1], axis=0)), [idxbs[ex_], h2ob], [xeb[xi]])
                    sc.dma("pool", lambda e, st=st, ex_=ex_, xi=xi: e.indirect_dma_start(out=gat[xi][:, :], out_offset=None, in_=aff_send[:, :],
                                                                                         in_offset=bass.IndirectOffsetOnAxis(ap=gidx[:, st, ex_:ex_ + 1], axis=0)), [idxbs[ex_], affsb], [gatb[xi]])

            idx_dve(0); idx_pe(0); gathers(0)
            idx_dve(1); idx_pe(1); gathers(1)
            yn = 0
            fcn = 0
            for ex_ in range(NE):
                i = ex_ % 2
                if ex_ + 2 < NE:
                    idx_dve(ex_ + 2)
                for st in range(3):
                    xi = i * 3 + st
                    for k in range(8):
                        sc.op("pe", lambda e, xi=xi, k=k: e.transpose(out=pst[:, k * 128:(k + 1) * 128], in_=xe[xi][:, k * 128:(k + 1) * 128], identity=identb[:]),
                              [xeb[xi], cb], [pstb])
                    sc.op("act", lambda e, st=st: e.activation(out=xeT[:, :, st * 128:(st + 1) * 128], in_=pst[:, :].rearrange("p (k t) -> p k t", k=8), func=AF.Copy),
                          [pstb], [xeTb])
                for fc in range(8):
                    ba, bb = (0, 1) if fcn % 2 == 0 else (2, 6)
                    si = fcn % 2; fcn += 1
                    for k in range(8):
                        sc.op("pe", lambda e, fc=fc, k=k, i=i, ba=ba: e.matmul(ps[ba][:, 0:CAP], lhsT=wgs[i][:, k, fc * 128:(fc + 1) * 128], rhs=xeT[:, k, :], start=(k == 0), stop=(k == 7)),
                              [wgb[i], xeTb], [psb[ba]])
                    for k in range(8):
                        sc.op("pe", lambda e, fc=fc, k=k, i=i, bb=bb: e.matmul(ps[bb][:, 0:CAP], lhsT=wus[i][:, k, fc * 128:(fc + 1) * 128], rhs=xeT[:, k, :], start=(k == 0), stop=(k == 7)),
                              [wub[i], xeTb], [psb[bb]])
                    sc.op("act", lambda e, ba=ba, si=si: e.activation(out=sils[si][:], in_=ps[ba][:, 0:CAP], func=AF.Silu), [psb[ba]], [silb[si]])
                    sc.op("dve", lambda e, fc=fc, bb=bb, si=si: e.tensor_tensor(out=hh_[:, fc, :], in0=sils[si][:], in1=ps[bb][:, 0:CAP], op=ALU.mult), [silb[si], psb[bb]], [hhb])
                for st in range(3):
                    xi = i * 3 + st
                    yi = yn % 2; yn += 1
                    for dh in range(2):
                        for fc in range(8):
                            sc.op("pe", lambda e, st=st, dh=dh, fc=fc, i=i: e.matmul(ps[3 + dh][:, :], lhsT=hh_[:, fc, st * 128:(st + 1) * 128], rhs=wds[i][:, fc, dh * 512:(dh + 1) * 512],
                                                                                    start=(fc == 0), stop=(fc == 7)), [hhb, wdb[i]], [psb[3 + dh]])
                        sc.op("dve", lambda e, xi=xi, dh=dh, yi=yi, ex_=ex_: e.scalar_tensor_tensor(out=ye[yi][:, dh * 512:(dh + 1) * 512], in0=ps[3 + dh][:, :], scalar=gat[xi][:, ex_:ex_ + 1],
                                                                                                  in1=gtmb[:, dh * 512:(dh + 1) * 512], op0=ALU.mult, op1=ALU.mult),
                              [psb[3 + dh], gatb[xi], cb], [yeb[yi]])
                    sc.dma("pool", lambda e, st=st, yi=yi, ex_=ex_: e.indirect_dma_start(out=yacc[:, :], out_offset=bass.IndirectOffsetOnAxis(ap=sidx[:, st, ex_:ex_ + 1], axis=0),
                                                                                         in_=ye[yi][:, :], in_offset=None, compute_op=ALU.add), [yeb[yi], idxbs[ex_]], [yaccb])
                if ex_ + 2 < NE:
                    idx_pe(ex_ + 2)
                    gathers(ex_ + 2)
                    load_expert(ex_ + 2)
            fo = [sb(p4, f"fo{i}", [128, 1024]) for i in range(4)]; fob = [Buf() for _ in range(4)]
            fj = sb(p4, "fj", [128, 1024]); fjb = Buf()
            fss = [sb(p4, f"fs{i}", [128, 4]) for i in range(4)]; fsbs = [Buf() for _ in range(4)]
            for j in range(16):
                i = j % 4
                fs, fsb = fss[i], fsbs[i]
                sc.dma("sp", lambda e, j=j, i=i: e.dma_start(out=fo[i][:], in_=yacc[j * 128:(j + 1) * 128, :]), [yaccb], [fob[i]])
                sc.op("act", lambda e, i=i, fs=fs: e.activation(out=fj[:], in_=fo[i][:], func=AF.Square, accum_out=fs[:, 0:1]), [fob[i]], [fjb, fsb])
                sc.op("dve", lambda e, fs=fs: e.tensor_scalar(out=fs[:, 1:2], in0=fs[:, 0:1], scalar1=1.0 / D, scalar2=EPS, op0=ALU.mult, op1=ALU.add), [fsb], [fsb])
                sc.op("act", lambda e, fs=fs: e.activation(out=fs[:, 1:2], in_=fs[:, 1:2], func=AF.Sqrt), [fsb], [fsb])
                sc.op("dve", lambda e, fs=fs: e.reciprocal(out=fs[:, 2:3], in_=fs[:, 1:2]), [fsb], [fsb])
                sc.op("dve", lambda e, i=i, fs=fs: e.scalar_tensor_tensor(out=fo[i][:], in0=fo[i][:], scalar=fs[:, 2:3], in1=gfinb[:], op0=ALU.mult, op1=ALU.mult), [fob[i], fsb, cb], [fob[i]])
                sc.dma("pool", lambda e, j=j, i=i: e.dma_start(out=y_d[j * 128:(j + 1) * 128, :], in_=fo[i][:]), [fob[i]], [])
            sc.barrier(final=True)
    return nc


def _rope_cols():
    rc = np.zeros((128, 8), np.float32)
    inv_m = (1.0 / (500000.0 ** (np.arange(0, 32, 2, dtype=np.float32) / 32))).astype(np.float32)
    inv_d = (1.0 / (500000.0 ** (np.arange(0, 16, 2, dtype=np.float32) / 16))).astype(np.float32)
    hp = np.float32(np.pi / 2)
    rc[:, 1] = hp
    rc[64:80, 0] = inv_m; rc[80:96, 0] = inv_m
    rc[64:80, 2] = -inv_m; rc[80:96, 2] = inv_m
    rc[:, 5] = hp
    for base in (0, 64):
        rc[base:base + 8, 4] = inv_d; rc[base + 8:base + 16, 4] = inv_d
        rc[base:base + 8, 6] = -inv_d; rc[base + 8:base + 16, 6] = inv_d
    return rc


def _masks():
    k = np.arange(128)[:, None]
    q = np.arange(512)[None, :]
    out = np.zeros((128, 20, 512), np.float32)
    for j in range(20):
        rel = 128 * j - 1024 + k - q
        m = np.zeros((128, 512), np.float32)
        for r in (1, 4, 16):
            m += ((rel % r == 0) & (np.abs(rel) <= 64 * r)).astype(np.float32)
        out[:, j, :] = np.where(m > 0, 8.0 * np.log(np.maximum(m, 1.0)), -30000.0)
    return out.astype(ml_dtypes.bfloat16)


def _col(v, n):
    return np.ascontiguousarray(np.asarray(v, np.float32).reshape(n, 128).T)


_NC_CACHE = {}


def kernel(x, c, positions, w_ada, b_ada, g_mix, w_in, g_q, w_uq, g_kv, w_ukv, w_out,
           g_ffn, w_router, b_router, w_gate, w_up, w_down, g_final):
    x = np.asarray(x, np.float32); c = np.asarray(c, np.float32)
    positions = np.asarray(positions, np.int32)
    w_in0 = np.asarray(w_in, np.float32)[0]
    o3 = 672
    dq = w_in0[:, o3:o3 + 512]; dk = w_in0[:, o3 + 512:o3 + 1024]; dv = w_in0[:, o3 + 1024:o3 + 1536]
    kr = w_in0[:, 640:672]

    def swap_heads(w, hd, rot):
        w = w.reshape(D, -1, hd).copy()
        s = w.copy()
        s[:, :, 0:rot // 2] = w[:, :, rot // 2:rot]
        s[:, :, rot // 2:rot] = w[:, :, 0:rot // 2]
        return s.reshape(D, -1)

    win_l = np.zeros((D, WI_COLS), np.float32)
    win_l[:, CQ:CQ + 384] = w_in0[:, 0:384]
    win_l[:, CKV:CKV + 256] = w_in0[:, 384:640]
    win_l[:, DQ:DQ + 512] = dq; win_l[:, DQS:DQS + 512] = swap_heads(dq, 64, 16)
    win_l[:, DK:DK + 512] = dk; win_l[:, DKS:DKS + 512] = swap_heads(dk, 64, 16)
    win_l[:, DV:DV + 512] = dv
    win_l[:, KR + 64:KR + 96] = kr
    win_l[:, KRS + 64:KRS + 80] = kr[:, 16:32]; win_l[:, KRS + 80:KRS + 96] = kr[:, 0:16]
    wuq0 = np.asarray(w_uq, np.float32)[0]
    wuq_s = wuq0.reshape(384, 8, 96).copy()
    tmp = wuq_s.copy()
    wuq_s[:, :, 64:80] = tmp[:, :, 80:96]; wuq_s[:, :, 80:96] = tmp[:, :, 64:80]
    wuq_l = np.ascontiguousarray(np.concatenate([wuq0, wuq_s.reshape(384, 768)], axis=1))
    wukv0 = np.asarray(w_ukv, np.float32)[0].reshape(256, 8, 128)
    wukv_k = np.ascontiguousarray(wukv0[:, :, 0:64].reshape(256, 512))
    wukv_v = np.ascontiguousarray(wukv0[:, :, 64:128].reshape(256, 512))
    sel = np.zeros((128, 64), np.float32); sel[64, :] = 1.0
    tri = (np.arange(128)[:, None] <= np.arange(128)[None, :]).astype(np.float32)
    iota_s = np.broadcast_to(np.arange(CAP, dtype=np.float32), (128, CAP)).copy()
    slot_col = (np.arange(3)[None, :] * 128 + np.arange(128)[:, None]).astype(np.float32)
    shared = {
        "g_mix_col": _col(np.asarray(g_mix)[0], 8),
        "g_ffn_col": _col(np.asarray(g_ffn)[0], 8),
        "g_final": np.asarray(g_final, np.float32),
        "w_in_l": win_l,
        "g_q_col": _col(np.asarray(g_q)[0], 3),
        "g_kv_col": _col(np.asarray(g_kv)[0], 2),
        "w_uq_l": wuq_l, "w_ukv_k": wukv_k, "w_ukv_v": wukv_v,
        "w_out": np.ascontiguousarray(np.asarray(w_out, np.float32)[0]),
        "w_router": np.ascontiguousarray(np.asarray(w_router, np.float32)[0]),
        "b_router": np.ascontiguousarray(np.asarray(b_router, np.float32)[0]),
        "w_gate": np.ascontiguousarray(np.asarray(w_gate, np.float32)[0]),
        "w_up": np.ascontiguousarray(np.asarray(w_up, np.float32)[0]),
        "w_down": np.ascontiguousarray(np.asarray(w_down, np.float32)[0]),
        "ident_f": np.eye(128, dtype=np.float32), "tri": tri, "sel": sel, "iota_s": iota_s, "slot_col": slot_col,
        "masks": _masks(), "rope_cols": _rope_cols(),
            }
    wada0 = np.asarray(w_ada, np.float32)[0]
    bada0 = np.asarray(b_ada, np.float32)[0]
    in_maps = []
    for core in range(NCORES):
        b, qtr = core // 4, core % 4
        m = dict(shared)
        lo_t = qtr * OWN - 1024
        xw = np.zeros((4096, D), np.float32)
        pw = np.zeros((4096,), np.int32)
        vw = np.zeros((4096,), np.float32)
        s0, s1 = max(lo_t, 0), min(lo_t + 4096, S)
        xw[s0 - lo_t:s1 - lo_t] = x[b, s0:s1]
        pw[s0 - lo_t:s1 - lo_t] = positions[b, s0:s1]
        vw[s0 - lo_t:s1 - lo_t] = 1.0
        m["x_win"] = xw
        m["pos_win"] = pw
        m["valid_col"] = np.ascontiguousarray(vw.reshape(32, 128).T)
        m["c_col"] = _col(c[b], 8)
        m["w_ada_q"] = np.ascontiguousarray(wada0[:, qtr * 1536:(qtr + 1) * 1536])
        m["b_ada_colq"] = _col(bada0[qtr * 1536:(qtr + 1) * 1536], 12)
        in_maps.append(m)
    if "nc" not in _NC_CACHE:
        _NC_CACHE["nc"] = build_program()
    res = run_bass_kernel_spmd(_NC_CACHE["nc"], in_maps, core_ids=list(range(NCORES)))
    _NC_CACHE["last"] = res.results
    out = np.zeros((2, S, D), np.float32)
    for core in range(NCORES):
        b, qtr = core // 4, core % 4
        out[b, qtr * OWN:(qtr + 1) * OWN] = np.asarray(res.results[core]["y"], np.float32)
    return out


if __name__ == "__main__":
    import time
    t0 = time.time()
    nc = build_program()
    print("build ok", time.time() - t0)
```

```python
import contextlib
import os
import numpy as np
import ml_dtypes
import concourse.bass as bass
import concourse.mybir as mybir
from concourse.bass_utils import run_bass_kernel_spmd

F32 = mybir.dt.float32
BF16 = mybir.dt.bfloat16
I32 = mybir.dt.int32
ALU = mybir.AluOpType
AF = mybir.ActivationFunctionType

S = 8192
D = 1024
NG = S // 512
NW = 8
NO = 4
NCORES = 8
OWN = 2048
CAP = 384
NE = 16
EPS = 1e-6
TWO_PI = 6.283185307179586
DK, DKS, DV, KR, KRS, CQ, CKV, DQ, DQS, WI_COLS = 0, 512, 1024, 1536, 1632, 1728, 2112, 2368, 2880, 3392
N_BISECT = 25


class Buf:
    __slots__ = ("w", "r")

    def __init__(self):
        self.w = None
        self.r = []


class Sched:
    def __init__(self, nc, es):
        self.nc = nc
        self.eng = {"pe": nc.tensor, "act": nc.scalar, "dve": nc.vector, "pool": nc.gpsimd, "sp": nc.sync}
        self.csem = {e: es.enter_context(nc.semaphore("c_" + e)) for e in ("pe", "act", "dve", "pool")}
        self.ccnt = {e: 0 for e in self.csem}
        self.dsem = {"sp": [es.enter_context(nc.semaphore(f"dsp{i}")) for i in range(8)],
                     "pool": [es.enter_context(nc.semaphore(f"dpl{i}")) for i in range(6)],
                     "pre": [es.enter_context(nc.semaphore(f"dpre{i}")) for i in range(4)]}
        self.dcnt = {}
        self.dnext = {"sp": 0, "pool": 0, "pre": 0}
        self.seen = {e: {} for e in self.eng}
        self.sems = {}

    def _wait(self, e, tok):
        sem, val = tok
        key = id(sem)
        if self.seen[e].get(key, 0) >= val:
            return
        self.eng[e].wait_ge(sem, val)
        self.seen[e][key] = val

    def _deps(self, e, reads, writes):
        toks = []
        for b in reads:
            if b.w is not None:
                toks.append(b.w)
        for b in writes:
            if b.w is not None:
                toks.append(b.w)
            toks.extend(b.r)
        for t in toks:
            if e == "pe" and t[0] is self.csem["pe"]:
                continue
            self._wait(e, t)

    def _commit(self, tok, reads, writes):
        for b in reads:
            b.r.append(tok)
        for b in writes:
            b.w = tok
            b.r = []

    def op(self, e, fn, reads=(), writes=()):
        self._deps(e, reads, writes)
        ins = fn(self.eng[e])
        self.ccnt[e] += 1
        sem = self.csem[e]
        ins.then_inc(sem, 1)
        tok = (sem, self.ccnt[e])
        self.sems[id(sem)] = tok
        self._commit(tok, reads, writes)

    def dma(self, q, fn, reads=(), writes=(), ring=None):
        rname = ring or q
        ring = self.dsem[rname]
        sem = ring[self.dnext[rname] % len(ring)]
        self.dnext[rname] += 1
        cnt = self.dcnt.get(id(sem), 0)
        if cnt:
            self._wait(q, (sem, 16 * cnt))
        self._deps(q, reads, writes)
        try:
            ins = fn(self.eng[q])
        except ValueError as ex:
            print("DMA FAIL", ex, "q", q, "dnext", self.dnext, flush=True)
            for trial in range(3):
                try:
                    ins = fn(self.eng[q]); print(" retry ok", trial, flush=True); break
                except ValueError as ex2:
                    print(" retry fail", trial, ex2, flush=True)
            else:
                raise
        ins.then_inc(sem, 16)
        self.dcnt[id(sem)] = cnt + 1
        tok = (sem, 16 * (cnt + 1))
        self.sems[id(sem)] = tok
        self._commit(tok, reads, writes)

    def dma_own(self, es, q, fn, reads=(), writes=()):
        self.nown = getattr(self, "nown", 0) + 1
        sem = es.enter_context(self.nc.semaphore(f"down{self.nown}"))
        self._deps(q, reads, writes)
        ins = fn(self.eng[q])
        ins.then_inc(sem, 16)
        tok = (sem, 16)
        self.csems_coll = getattr(self, "csems_coll", [])
        self.csems_coll.append(tok)
        self._commit(tok, reads, writes)

    def coll(self, es, fn, reads=(), writes=()):
        self.ncoll = getattr(self, "ncoll", 0) + 1
        sem = es.enter_context(self.nc.semaphore(f"ccoll{self.ncoll}"))
        self._deps("pool", reads, writes)
        ins = fn(self.eng["pool"])
        ins.then_inc(sem)
        tok = (sem, 1)
        self.csems_coll = getattr(self, "csems_coll", [])
        self.csems_coll.append(tok)
        self._commit(tok, reads, writes)

    def barrier(self, final=False):
        toks = list(self.sems.values())
        if final:
            toks += getattr(self, "csems_coll", [])
        for e in self.eng:
            for t in toks:
                self._wait(e, t)


def build_program():
    nc = bass.Bass("TRN2", target_bir_lowering=False)
    es = contextlib.ExitStack()

    def din(name, shape, dt=F32):
        return nc.dram_tensor(name, list(shape), dt, kind="ExternalInput")

    dbg = bool(os.environ.get("KDEBUG"))

    def dscr(name, shape, dt):
        if dbg:
            return nc.dram_tensor(name, list(shape), dt, kind="ExternalOutput")
        return nc.dram_tensor(name, list(shape), dt)

    x_d = din("x_win", [4096, D])
    ccol_d = din("c_col", [128, 8])
    pos_d = din("pos_win", [4096], I32)
    valid_d = din("valid_col", [128, 32])
    wada_d = din("w_ada_q", [D, 1536])
    bada_d = din("b_ada_colq", [128, 12])
    gmix_d = din("g_mix_col", [128, 8])
    gffn_d = din("g_ffn_col", [128, 8])
    gfin_d = din("g_final", [D])
    win_d = din("w_in_l", [D, WI_COLS])
    gq_d = din("g_q_col", [128, 3])
    gkv_d = din("g_kv_col", [128, 2])
    wuq_d = din("w_uq_l", [384, 1536])
    wukvk_d = din("w_ukv_k", [256, 512])
    wukvv_d = din("w_ukv_v", [256, 512])
    wout_d = din("w_out", [D, D])
    wr_d = din("w_router", [D, NE])
    br_d = din("b_router", [NE])
    wg_d = din("w_gate", [NE, D, D])
    wu_d = din("w_up", [NE, D, D])
    wd_d = din("w_down", [NE, D, D])
    identf_d = din("ident_f", [128, 128])
    tri_d = din("tri", [128, 128])
    sel_d = din("sel", [128, 64])
    iota_d = din("iota_s", [128, CAP])
    slotc_d = din("slot_col", [128, 3])
    masks_d = din("masks", [128, 20, 512], BF16)
    ropec_d = din("rope_cols", [128, 8])
    y_d = nc.dram_tensor("y", [OWN, D], F32, kind="ExternalOutput")

    qT_all = dscr("qT_own", [8, 96, OWN], BF16)
    kT_send = [dscr(f"kT_send{c}", [192, OWN], BF16) for c in range(4)]
    kT_g = [dscr(f"kT_g{c}", [4 * 192, OWN], BF16) for c in range(4)]
    vaug_send = [dscr(f"vaug_send{c}", [512, 520], BF16) for c in range(4)]
    vaug_all = [dscr(f"vaug_g{c}", [2048, 520], BF16) for c in range(4)]
    dqT_all = dscr("dqT_own", [4, 128, OWN], BF16)
    dkT_all = dscr("dkT_win", [4, 128, 4096], BF16)
    dvaug_all = dscr("dvaug_win", [4096, 520], BF16)
    mixT_all = dscr("mixT_own", [D, OWN], BF16)
    h2_all = dscr("h2_own", [OWN, D], BF16)
    aff_send = dscr("aff_send", [OWN, NE], F32)
    aff_all = dscr("aff_g", [S, NE], F32)
    yacc = dscr("yacc", [OWN + CAP, D], F32)
    mod_send = dscr("mod_send", [128, 12], F32)
    mod_g = dscr("mod_g", [512, 12], F32)
    w16 = {m: dscr(f"w16{m}", [NE, D, D], BF16) for m in "gud"}

    def sb(stack, name, shape, dt=F32):
        return stack.enter_context(nc.sbuf_tensor(name, list(shape), dt))

    with es:
        sc = Sched(nc, es)
        ps = [es.enter_context(nc.psum_tensor(f"ps{i}", [128, 512], F32)) for i in range(6)]
        psb = [Buf() for _ in range(7)]
        pstb = Buf()


        identf = sb(es, "identf", [128, 128]); identb = sb(es, "identb", [128, 128], BF16)
        onesb = sb(es, "onesb", [128, 128], BF16); onesf = sb(es, "onesf", [128, 128])
        ropec = sb(es, "ropec", [128, 8])
        modc = sb(es, "modc", [128, 6, 8])
        g1c = sb(es, "g1c", [128, 8])
        gtab = sb(es, "gtab", [128, 1024]); g2b = sb(es, "g2b", [128, 1024])
        shmb = sb(es, "shmb", [128, 1024]); gtmb = sb(es, "gtmb", [128, 1024])
        gfinb = sb(es, "gfinb", [128, 1024])
        validc = sb(es, "validc", [128, 32]); affo = sb(es, "affo", [128, 16, NE]); affob = Buf()
        kgb = Buf(); vgb = Buf(); affgb = Buf(); affsb = Buf()
        preb = {(m, ex): Buf() for m in "gud" for ex in range(NE)}
        cb = Buf()

        def load(q, dst, src, bufs_w, bufs_r=()):
            sc.dma(q, lambda e: e.dma_start(out=dst, in_=src), reads=bufs_r, writes=bufs_w)

        load("sp", identf[:], identf_d[:, :], [cb])
        load("sp", ropec[:], ropec_d[:, :], [cb])
        load("sp", validc[:], valid_d[:, :], [cb])
        load("sp", gfinb[:], gfin_d.ap().partition_broadcast(128), [cb])
        sc.op("dve", lambda e: e.tensor_copy(out=identb[:], in_=identf[:]), [cb], [cb])
        sc.op("dve", lambda e: e.memset(onesb[:], 1.0), [], [cb])
        sc.op("dve", lambda e: e.memset(onesf[:], 1.0), [], [cb])

        pw = contextlib.ExitStack()
        wi = sb(pw, "wi", [128, 8, WI_COLS], BF16)
        wuq = sb(pw, "wuq", [128, 3, 1536], BF16)
        wkk = sb(pw, "wkk", [128, 2, 512], BF16); wkv = sb(pw, "wkv", [128, 2, 512], BF16)
        gqc = sb(pw, "gqc", [128, 3]); gkvc = sb(pw, "gkvc", [128, 2])
        wb = Buf()
        wba = Buf()

        def load_proj_weights():
            for c0 in range(0, WI_COLS, 848):
                load("pool", wi[:, :, c0:c0 + 848], win_d.ap()[:, c0:c0 + 848].rearrange("(k p) n -> p k n", p=128), [wba if c0 < 1696 else wb])
            load("pool", wuq[:], wuq_d.ap().rearrange("(k p) n -> p k n", p=128), [wb])
            load("pool", wkk[:], wukvk_d.ap().rearrange("(k p) n -> p k n", p=128), [wb])
            load("pool", wkv[:], wukvv_d.ap().rearrange("(k p) n -> p k n", p=128), [wb])
            load("sp", gqc[:], gq_d[:, :], [wb]); load("sp", gkvc[:], gkv_d[:, :], [wb])

        with contextlib.ExitStack() as p0:
            ccol = sb(p0, "ccol", [128, 8]); scol = sb(p0, "scol", [128, 8])
            badac = sb(p0, "badac", [128, 12]); gmixc = sb(p0, "gmixc", [128, 8]); gffnc = sb(p0, "gffnc", [128, 8])
            was = [sb(p0, f"wa{i}", [128, 8, 768], BF16) for i in range(2)]; wabs = [Buf(), Buf()]
            scolb = sb(p0, "scolb", [128, 8], BF16)
            tmpc = sb(p0, "tmpc", [128, 8]); g2c = sb(p0, "g2c", [128, 8])
            bsrc = sb(p0, "bsrc", [128, 128]); bsb = Buf()
            modp = sb(p0, "modp", [128, 12]); modpb = Buf(); msb = Buf(); mgb_ = Buf()
            load("sp", ccol[:], ccol_d[:, :], [cb]); load("sp", badac[:], bada_d[:, :], [cb])
            load("sp", gmixc[:], gmix_d[:, :], [cb]); load("sp", gffnc[:], gffn_d[:, :], [cb])
            sc.op("act", lambda e: e.activation(out=scol[:], in_=ccol[:], func=AF.Silu), [cb], [cb])
            sc.op("dve", lambda e: e.tensor_copy(out=scolb[:], in_=scol[:]), [cb], [cb])
            for hf in range(2):
                load("pool", was[hf][:], wada_d.ap()[:, hf * 768:(hf + 1) * 768].rearrange("(k p) n -> p k n", p=128), [wabs[hf]])
            load_proj_weights()
            for hf in range(2):
                wa, wab = was[hf], wabs[hf]
                for cc in range(6):
                    for k in range(8):
                        sc.op("pe", lambda e, cc=cc, k=k, wa=wa, hf=hf: e.matmul(ps[5][:, hf * 6 + cc:hf * 6 + cc + 1], lhsT=wa[:, k, cc * 128:(cc + 1) * 128],
                                                                                rhs=scolb[:, k:k + 1], start=(k == 0), stop=(k == 7)),
                              [wab, cb], [psb[5]])
            sc.op("dve", lambda e: e.tensor_tensor(out=modp[:], in0=ps[5][:, 0:12], in1=badac[:], op=ALU.add), [psb[5], cb], [modpb])
            sc.dma("sp", lambda e: e.dma_start(out=mod_send[:, :], in_=modp[:]), [modpb], [msb])
            sc.coll(es, lambda e: e.collective_compute("AllGather", ALU.bypass, replica_groups=[[0, 1, 2, 3], [4, 5, 6, 7]],
                                                       ins=[mod_send.ap().opt()], outs=[mod_g.ap().opt()]), [msb], [mgb_])
            sc.dma("sp", lambda e: e.dma_start(out=modc[:].rearrange("p m k -> p (m k)").rearrange("p (r j) -> p r j", r=4),
                                               in_=mod_g.ap().rearrange("(r p) j -> p r j", p=128)), [mgb_], [cb])
            sc.op("dve", lambda e: e.tensor_scalar(out=tmpc[:], in0=modc[:, 1, :], scalar1=1.0, scalar2=None, op0=ALU.add), [cb], [cb])
            sc.op("dve", lambda e: e.tensor_tensor(out=g1c[:], in0=tmpc[:], in1=gmixc[:], op=ALU.mult), [cb], [cb])
            sc.op("dve", lambda e: e.tensor_scalar(out=tmpc[:], in0=modc[:, 4, :], scalar1=1.0, scalar2=None, op0=ALU.add), [cb], [cb])
            sc.op("dve", lambda e: e.tensor_tensor(out=g2c[:], in0=tmpc[:], in1=gffnc[:], op=ALU.mult), [cb], [cb])
            for src_col, dst in ((modc[:, 2, :], gtab), (g2c[:], g2b), (modc[:, 3, :], shmb), (modc[:, 5, :], gtmb)):
                for half in range(2):
                    for kk in range(4):
                        k = half * 4 + kk
                        sc.op("dve", lambda e, k=k, src_col=src_col: e.tensor_scalar(out=bsrc[:], in0=onesf[:], scalar1=src_col[:, k:k + 1],
                                                                                    scalar2=None, op0=ALU.mult), [cb], [bsb])
                        sc.op("pe", lambda e, kk=kk: e.matmul(ps[5][:, kk * 128:(kk + 1) * 128], lhsT=bsrc[:], rhs=identf[:], start=True, stop=True),
                              [bsb, cb], [psb[5]])
                    sc.op("dve", lambda e, half=half, dst=dst: e.tensor_copy(out=dst[:, half * 512:(half + 1) * 512], in_=ps[5][:, :]), [psb[5]], [cb])
        sc.barrier()

        with contextlib.ExitStack() as p1:

            xss = [sb(p1, f"xs{i}", [128, 4, 1024], BF16) for i in range(2)]; xsb = [Buf(), Buf()]
            xgs = [sb(p1, "xg0", [128, 4, 1024])] * 2; xgbs = [Buf()] * 2
            xjunk = sb(p1, "xjunk", [128, 1024], BF16); xjb = Buf()
            ssc = [sb(p1, f"ssc{i}", [128, 12]) for i in range(2)]; sscb = [Buf(), Buf()]
            sq = sb(p1, "sq", [128, 3, 512], BF16); sqb = Buf()
            hT = sb(p1, "hT", [128, 8, 512], BF16); hTb = Buf()
            rstd = sb(p1, "rstd", [128, 512]); rstdb = Buf()
            posi = sb(p1, "posi", [128, 512], I32); posf = sb(p1, "posf", [128, 512]); posb = Buf()
            ang = sb(p1, "ang", [128, 512]); tq = sb(p1, "tq", [128, 512]); ti = sb(p1, "ti", [128, 512], I32); angb = Buf()
            tabsets = [[sb(p1, f"tab{j}_{i}", [128, 512]) for i in range(4)] for j in range(2)]; tabbsets = [[Buf() for _ in range(4)] for _ in range(2)]
            cT = sb(p1, "cT", [128, 3, 512]); cTb = Buf()
            cn = sb(p1, "cn", [128, 3, 512], BF16); cnb = Buf()
            kvn = sb(p1, "kvn", [128, 2, 512], BF16); kvnb = Buf()
            t1s = [sb(p1, f"t1_{i}", [128, 512]) for i in range(2)]; t2s = [sb(p1, f"t2_{i}", [128, 512]) for i in range(2)]
            t1bs = [Buf() for _ in range(2)]; t2bs = [Buf() for _ in range(2)]; rcn = [0]
            pstbs = [Buf(), Buf()]
            psts = [p1.enter_context(nc.psum_tensor(f"pstA{i}", [128, 1024], BF16)) for i in range(2)]
            qsbs = [sb(p1, f"qsb{i}", [96, 512], BF16) for i in range(2)]; qsbbs = [Buf(), Buf()]
            ksbs = [sb(p1, f"ksb{i}", [96, 512], BF16) for i in range(2)]; ksbbs = [Buf(), Buf()]
            krs = sb(p1, "krs", [96, 512], BF16); krsb = Buf()
            dqs = sb(p1, "dqs", [128, 4, 512], BF16); dqsb = Buf()
            dks = sb(p1, "dks", [128, 4, 512], BF16); dksb = Buf()
            va = sb(p1, "va", [128, 8, 65], BF16); vab = Buf()
            dva = sb(p1, "dva", [128, 8, 65], BF16); dvab = Buf()
            ksendb = Buf(); vsendb = Buf()
            sc.op("pool", lambda e: e.memset(va[:], 1.0), [], [vab])
            sc.op("pool", lambda e: e.memset(dva[:], 1.0), [], [dvab])

            def rsqrt_from(psrc, dst, dstb, scale, rd):
                sc.op("dve", lambda e: e.tensor_scalar(out=dst, in0=psrc, scalar1=scale, scalar2=EPS, op0=ALU.mult, op1=ALU.add), rd, [dstb])
                sc.op("act", lambda e: e.activation(out=dst, in_=dst, func=AF.Sqrt), [dstb], [dstb])
                sc.op("dve", lambda e: e.reciprocal(out=dst, in_=dst), [dstb], [dstb])

            def make_table(si, i, invc, phc):
                sc.op("dve", lambda e: e.tensor_scalar(out=ang[:], in0=posf[:], scalar1=ropec[:, invc:invc + 1], scalar2=ropec[:, phc:phc + 1],
                                                      op0=ALU.mult, op1=ALU.add), [posb, cb], [angb])
                sc.op("dve", lambda e: e.tensor_scalar(out=tq[:], in0=ang[:], scalar1=1.0 / TWO_PI, scalar2=None, op0=ALU.mult), [angb], [angb])
                sc.op("dve", lambda e: e.tensor_copy(out=ti[:], in_=tq[:]), [angb], [angb])
                sc.op("dve", lambda e: e.tensor_copy(out=tq[:], in_=ti[:]), [angb], [angb])
                sc.op("dve", lambda e: e.scalar_tensor_tensor(out=ang[:], in0=tq[:], scalar=-TWO_PI, in1=ang[:], op0=ALU.mult, op1=ALU.add), [angb], [angb])
                sc.op("dve", lambda e: e.tensor_scalar(out=ang[:], in0=ang[:], scalar1=-3.14159, scalar2=3.14159, op0=ALU.max, op1=ALU.min), [angb], [angb])
                sc.op("act", lambda e: e.activation(out=tabsets[si][i][:], in_=ang[:], func=AF.Sin), [angb], [tabbsets[si][i]])

            def fm_chain(pi, col0, M):
                wdeps = [wba] if col0 + M <= 1696 else [wba, wb]
                for k in range(8):
                    sc.op("pe", lambda e, k=k: e.matmul(ps[pi][0:M, :], lhsT=wi[:, k, col0:col0 + M], rhs=hT[:, k, :], start=(k == 0), stop=(k == 7)),
                          wdeps + [hTb], [psb[pi]])

            dvas = [dva, sb(p1, "dva2", [128, 8, 65], BF16)]; dvabs = [dvab, Buf()]
            rr = [0]

            def bank():
                rr[0] = (rr[0] + 1) % 6
                return rr[0]

            def fm_chain(pi, col0, M):
                wdeps = [wba] if col0 + M <= 1696 else [wba, wb]
                for k in range(8):
                    sc.op("pe", lambda e, k=k: e.matmul(ps[pi][0:M, :], lhsT=wi[:, k, col0:col0 + M], rhs=hT[:, k, :], start=(k == 0), stop=(k == 7)),
                          wdeps + [hTb], [psb[pi]])

            def fe_a(wg):
                tok0 = wg * 512
                own = 2 <= wg < 6
                gi = wg % 2
                xg, xgb, xs, xsbuf, ss, ssb = xgs[gi], xgbs[gi], xss[gi], xsb[gi], ssc[gi], sscb[gi]
                load("sp", xg[:], x_d.ap()[tok0:tok0 + 512, :].rearrange("(t p) d -> p t d", p=128), [xgb])
                load("sp", posi[:], pos_d.ap()[tok0:tok0 + 512].partition_broadcast(128), [posb])
                sc.op("dve", lambda e: e.tensor_copy(out=posf[:], in_=posi[:]), [posb], [posb])
                if own:
                    make_table(gi, 0, 0, 1); make_table(gi, 1, 2, 3)
                make_table(gi, 2, 4, 5); make_table(gi, 3, 6, 7)
                for t in range(4):
                    sc.op("act", lambda e, t=t, xg=xg, ss=ss: e.activation(out=xjunk[:], in_=xg[:, t, :], func=AF.Square, accum_out=ss[:, t:t + 1]), [xgb], [xjb, ssb])
                sc.op("dve", lambda e, ss=ss: e.tensor_scalar(out=ss[:, 4:8], in0=ss[:, 0:4], scalar1=1.0 / D, scalar2=EPS, op0=ALU.mult, op1=ALU.add), [ssb], [ssb])
                sc.op("act", lambda e, ss=ss: e.activation(out=ss[:, 4:8], in_=ss[:, 4:8], func=AF.Sqrt), [ssb], [ssb])
                sc.op("dve", lambda e, ss=ss: e.reciprocal(out=ss[:, 8:12], in_=ss[:, 4:8]), [ssb], [ssb])
                for t in range(4):
                    sc.op("dve", lambda e, t=t, xg=xg, xs=xs, ss=ss: e.tensor_scalar(out=xs[:, t, :], in0=xg[:, t, :], scalar1=ss[:, 8 + t:9 + t], scalar2=None, op0=ALU.mult), [xgb, ssb], [xsbuf])

            def fe_b(wg):
                gi = wg % 2
                xs, xsbuf = xss[gi], xsb[gi]
                for k in range(8):
                    hb = k % 2
                    for t in range(4):
                        sc.op("pe", lambda e, k=k, t=t, hb=hb, xs=xs: e.transpose(out=psts[hb][:, t * 128:(t + 1) * 128], in_=xs[:, t, k * 128:(k + 1) * 128], identity=identb[:]),
                              [xsbuf, cb], [pstbs[hb]])
                    sc.op("act", lambda e, k=k, hb=hb: e.activation(out=hT[:, k, :], in_=psts[hb][:, 0:512], func=AF.Identity, scale=g1c[:, k:k + 1], bias=modc[:, 0, k:k + 1]),
                          [pstbs[hb], cb], [hTb])

            def projections(wg):
                tok0 = wg * 512
                own = 2 <= wg < 6
                to = (wg - 2) * 512
                tabs, tabb = tabsets[wg % 2], tabbsets[wg % 2]

                def rope_combine(pa, pb_, lo, hi, tc, ts, dst, dstb):
                    ri = rcn[0] % 2; rcn[0] += 1
                    t1, t2, t1b, t2b = t1s[ri], t2s[ri], t1bs[ri], t2bs[ri]
                    sc.op("dve", lambda e: e.tensor_tensor(out=t1[lo:hi, :], in0=ps[pa][lo:hi, :], in1=tabs[tc][lo:hi, :], op=ALU.mult), [psb[pa], tabb[tc]], [t1b])
                    sc.op("dve", lambda e: e.tensor_tensor(out=t2[lo:hi, :], in0=ps[pb_][lo:hi, :], in1=tabs[ts][lo:hi, :], op=ALU.mult), [psb[pb_], tabb[ts]], [t2b])
                    sc.op("pool", lambda e: e.tensor_tensor(out=dst, in0=t1[lo:hi, :], in1=t2[lo:hi, :], op=ALU.add), [t1b, t2b], [dstb])

                if own:
                    for c in range(3):
                        bk = bank()
                        fm_chain(bk, CQ + c * 128, 128)
                        sc.op("act", lambda e, c=c, bk=bk: e.activation(out=cT[:, c, :], in_=ps[bk][:, :], func=AF.Copy), [psb[bk]], [cTb])
                        sc.op("act", lambda e, c=c, bk=bk: e.activation(out=sq[:, c, :], in_=ps[bk][:, :], func=AF.Square), [psb[bk]], [sqb])
                    bk = bank()
                    for c in range(3):
                        sc.op("pe", lambda e, c=c, bk=bk: e.matmul(ps[bk][:, :], lhsT=onesb[:], rhs=sq[:, c, :], start=(c == 0), stop=(c == 2)), [sqb, cb], [psb[bk]])
                    rsqrt_from(ps[bk][:, :], rstd[:], rstdb, 1.0 / 384, [psb[bk]])
                    for c in range(3):
                        sc.op("dve", lambda e, c=c: e.scalar_tensor_tensor(out=cn[:, c, :], in0=cT[:, c, :], scalar=gqc[:, c:c + 1], in1=rstd[:], op0=ALU.mult, op1=ALU.mult),
                              [cTb, rstdb, wb], [cnb])
                    for h in range(8):
                        ba, bb = bank(), bank()
                        for var, pi in ((0, ba), (768, bb)):
                            for c in range(3):
                                sc.op("pe", lambda e, h=h, var=var, pi=pi, c=c: e.matmul(ps[pi][0:96, :], lhsT=wuq[:, c, var + h * 96: var + (h + 1) * 96], rhs=cn[:, c, :],
                                                                                          start=(c == 0), stop=(c == 2)), [wb, cnb], [psb[pi]])
                        rope_combine(ba, bb, 0, 96, 0, 1, qsbs[h % 2][:, :], qsbbs[h % 2])
                        sc.dma("sp", lambda e, to=to, h=h: e.dma_start(out=qT_all[h, :, to:to + 512], in_=qsbs[h % 2][:]), [qsbbs[h % 2]], [])
                    for c in range(2):
                        bk = bank()
                        fm_chain(bk, CKV + c * 128, 128)
                        sc.op("act", lambda e, c=c, bk=bk: e.activation(out=cT[:, c, :], in_=ps[bk][:, :], func=AF.Copy), [psb[bk]], [cTb])
                        sc.op("act", lambda e, c=c, bk=bk: e.activation(out=sq[:, c, :], in_=ps[bk][:, :], func=AF.Square), [psb[bk]], [sqb])
                    bk = bank()
                    for c in range(2):
                        sc.op("pe", lambda e, c=c, bk=bk: e.matmul(ps[bk][:, :], lhsT=onesb[:], rhs=sq[:, c, :], start=(c == 0), stop=(c == 1)), [sqb, cb], [psb[bk]])
                    rsqrt_from(ps[bk][:, :], rstd[:], rstdb, 1.0 / 256, [psb[bk]])
                    for c in range(2):
                        sc.op("dve", lambda e, c=c: e.scalar_tensor_tensor(out=kvn[:, c, :], in0=cT[:, c, :], scalar=gkvc[:, c:c + 1], in1=rstd[:], op0=ALU.mult, op1=ALU.mult),
                              [cTb, rstdb, wb], [kvnb])
                    ba, bb = bank(), bank()
                    fm_chain(ba, KR, 96); fm_chain(bb, KRS, 96)
                    rope_combine(ba, bb, 64, 96, 0, 1, krs[64:96, :], krsb)
                    for h in range(8):
                        bk = bank()
                        for r in range(2):
                            sc.op("pe", lambda e, h=h, r=r, bk=bk: e.matmul(ps[bk][0:64, :], lhsT=wkk[:, r, h * 64:(h + 1) * 64], rhs=kvn[:, r, :], start=(r == 0), stop=(r == 1)),
                                  [wb, kvnb], [psb[bk]])
                        sc.op("act", lambda e, h=h, bk=bk: e.activation(out=ksbs[h % 2][0:64, :], in_=ps[bk][0:64, :], func=AF.Copy), [psb[bk]], [ksbbs[h % 2]])
                        sc.op("pool", lambda e, h=h: e.tensor_copy(out=ksbs[h % 2][64:96, :], in_=krs[64:96, :]), [krsb], [ksbbs[h % 2]])
                        sc.dma("sp", lambda e, to=to, h=h: e.dma_start(out=kT_send[h // 2][(h % 2) * 96:(h % 2 + 1) * 96, to:to + 512], in_=ksbs[h % 2][:]), [ksbbs[h % 2]], [ksendb])
                    for t in range(4):
                        bk = bank()
                        for r in range(2):
                            sc.op("pe", lambda e, t=t, r=r, bk=bk: e.matmul(ps[bk][:, :], lhsT=kvn[:, r, t * 128:(t + 1) * 128], rhs=wkv[:, r, :], start=(r == 0), stop=(r == 1)),
                                  [wb, kvnb], [psb[bk]])
                        sc.op("act", lambda e, bk=bk: e.activation(out=va[:, :, 0:64], in_=ps[bk][:, :].rearrange("p (h c) -> p h c", h=8), func=AF.Copy), [psb[bk]], [vab])
                        sc.dma("sp", lambda e, t=t, og=wg - 2: e.dma_start(out=vaug_send[og][t * 128:(t + 1) * 128, :], in_=va[:].rearrange("p h c -> p (h c)")), [vab], [vsendb])
                    for pr in range(4):
                        ba, bb = bank(), bank()
                        fm_chain(ba, DQ + pr * 128, 128); fm_chain(bb, DQS + pr * 128, 128)
                        rope_combine(ba, bb, 0, 128, 2, 3, dqs[:, pr, :], dqsb)
                    sc.dma("sp", lambda e, to=to: e.dma_start(out=dqT_all.ap()[:, :, to:to + 512].rearrange("h r t -> r h t"), in_=dqs[:]), [dqsb], [])
                for pr in range(4):
                    ba, bb = bank(), bank()
                    fm_chain(ba, DK + pr * 128, 128); fm_chain(bb, DKS + pr * 128, 128)
                    rope_combine(ba, bb, 0, 128, 2, 3, dks[:, pr, :], dksb)
                sc.dma("sp", lambda e, tok0=tok0: e.dma_start(out=dkT_all.ap()[:, :, tok0:tok0 + 512].rearrange("h r t -> r h t"), in_=dks[:]), [dksb], [])
                for t in range(4):
                    bk = bank()
                    for k in range(8):
                        sc.op("pe", lambda e, t=t, k=k, bk=bk: e.matmul(ps[bk][:, :], lhsT=hT[:, k, t * 128:(t + 1) * 128], rhs=wi[:, k, DV:DV + 512], start=(k == 0), stop=(k == 7)),
                              [wba, hTb], [psb[bk]])
                    vc = wg * 4 + t
                    dv_ = dvas[t % 2]; dvb_ = dvabs[t % 2]
                    sc.op("act", lambda e, bk=bk, vc=vc, dv_=dv_: e.activation(out=dv_[:, :, 0:64], in_=ps[bk][:, :].rearrange("p (h c) -> p h c", h=8), func=AF.Copy, scale=validc[:, vc:vc + 1]),
                          [psb[bk], cb], [dvb_])
                    sc.op("dve", lambda e, vc=vc, dv_=dv_: e.tensor_copy(out=dv_[:, :, 64], in_=validc[:, vc:vc + 1].broadcast_to([128, 8])), [cb], [dvb_])
                    sc.dma("sp", lambda e, t=t, tok0=tok0, dv_=dv_: e.dma_start(out=dvaug_all[tok0 + t * 128: tok0 + (t + 1) * 128, :],
                                                                          in_=dv_[:].rearrange("p h c -> p (h c)")), [dvb_], [])
            sc.op("pe", lambda e: e.matmul(ps[0][:, 0:128], lhsT=onesb[:], rhs=onesb[:, 0:128], start=True, stop=True), [cb], [psb[0]])
            fe_a(0); fe_b(0)
            for wg in range(NW):
                if wg + 1 < NW:
                    fe_a(wg + 1)
                projections(wg)
                if wg + 1 < NW:
                    fe_b(wg + 1)
            for c4 in range(4):
                sc.coll(es, lambda e, c4=c4: e.collective_compute("AllGather", ALU.bypass, replica_groups=[[0, 1, 2, 3], [4, 5, 6, 7]],
                                                                  ins=[kT_send[c4].ap().opt()], outs=[kT_g[c4].ap().opt()]), [ksendb], [kgb])
            for c4 in range(4):
                sc.coll(es, lambda e, c4=c4: e.collective_compute("AllGather", ALU.bypass, replica_groups=[[0, 1, 2, 3], [4, 5, 6, 7]],
                                                                  ins=[vaug_send[c4].ap().opt()], outs=[vaug_all[c4].ap().opt()]), [vsendb], [vgb])
        sc.barrier()
        pw.close()

        if os.environ.get("KSTOP") == "a1":
            sc.barrier(final=True)
            return nc
        pwo = contextlib.ExitStack()
        wo = sb(pwo, "wo", [128, 8, 1024], BF16); wob = Buf()
        with contextlib.ExitStack() as p2:
            kTs = [sb(p2, f"kT{i}", [128, S], BF16) for i in range(2)]; kTbs = [Buf(), Buf()]
            qTs = [sb(p2, f"qT{i}", [128, OWN], BF16) for i in range(2)]; qTbs = [Buf(), Buf()]
            vvs = [sb(p2, f"vv{i}", [128, 64, 65], BF16) for i in range(2)]; vvbs = [Buf(), Buf()]
            msk = sb(p2, "msk", [128, 20, 512], BF16); mskb = Buf()
            selt = sb(p2, "selt", [128, 64]);
            ps.append(p2.enter_context(nc.psum_tensor("ps6a", [128, 512], F32)))
            SB = [0, 1, 2, 6]
            pts = [sb(p2, f"pt{i}", [128, 512], BF16) for i in range(4)]; ptb = [Buf() for _ in range(4)]
            osb = [sb(p2, f"osb{i}", [65, 512]) for i in range(2)]; osbb = [Buf(), Buf()]
            rden = [sb(p2, f"rden{i}", [64, 512]) for i in range(2)]; rdenb = [Buf(), Buf()]
            oT = [sb(p2, f"oT{i}", [64, 512], BF16) for i in range(2)]; oTb = [Buf(), Buf()]
            mskbs = [Buf() for _ in range(4)]
            load("sp", selt[:], sel_d[:, :], [mskb])
            load("pool", wo[:], wout_d.ap().rearrange("(k p) n -> p k n", p=128), [wob])
            blk = 0
            pace = Buf()

            def attend(kT, kTb, qT, qTb, vv, vvb, nkt, kbase_fn, vidx_fn, lo, hi, scale, masked, row0, qb, blk):
                po = 3 + (blk % 2)
                LA = 2
                for kt in range(nkt + LA):
                    if kt < nkt:
                        si = kt % 4
                        kc = kbase_fn(kt)
                        sc.op("pe", lambda e, si=si, kc=kc: e.matmul(ps[SB[si]][:, :], lhsT=kT[lo:hi, kc:kc + 128], rhs=qT[lo:hi, qb * 512:(qb + 1) * 512], start=True, stop=not masked),
                              [kTb, qTb], [psb[SB[si]]])
                        if masked:
                            sc.op("pe", lambda e, si=si, kt=kt: e.matmul(ps[SB[si]][:, :], lhsT=identb[:], rhs=msk[:, kt, :], start=False, stop=True),
                                  [mskbs[kt // 5], cb], [psb[SB[si]]])
                        sc.op("act", lambda e, si=si: e.activation(out=pts[si][:], in_=ps[SB[si]][:, :], func=AF.Exp, scale=scale), [psb[SB[si]]], [ptb[si]])
                    if kt >= LA:
                        k0 = kt - LA
                        si = k0 % 4
                        vt = vidx_fn(k0)
                        sc.op("pe", lambda e, si=si, vt=vt, k0=k0: e.matmul(ps[po][0:65, :], lhsT=vv[:, vt, :], rhs=pts[si][:], start=(k0 == 0), stop=(k0 == nkt - 1)),
                              [vvb, ptb[si]], [psb[po]])
                oi = blk % 2
                sc.op("act", lambda e: e.activation(out=osb[oi][:], in_=ps[po][0:65, :], func=AF.Copy), [psb[po]], [osbb[oi]])
                sc.op("pe", lambda e: e.matmul(ps[5][0:64, :], lhsT=selt[0:65, :], rhs=osb[oi][:], start=True, stop=True), [osbb[oi], mskb], [psb[5]])
                sc.op("dve", lambda e: e.reciprocal(out=rden[oi][:], in_=ps[5][0:64, :]), [psb[5]], [rdenb[oi]])
                sc.op("dve", lambda e: e.tensor_tensor(out=oT[oi][:], in0=osb[oi][0:64, :], in1=rden[oi][:], op=ALU.mult), [osbb[oi], rdenb[oi]], [oTb[oi], pace])
                sc.dma("sp", lambda e: e.dma_start(out=mixT_all[row0:row0 + 64, qb * 512:(qb + 1) * 512], in_=oT[oi][:]), [oTb[oi]], [])

            qdil = [[sb(p2, f"qdil{i}{j}", [128, OWN], BF16) for j in range(2)] for i in range(2)]; qdilb = [Buf(), Buf()]
            for i in range(2):
                sc.op("dve", lambda e, i=i: e.memset(qdil[i][0][64:128, :], 0.0), [], [qdilb[i]])
                sc.op("dve", lambda e, i=i: e.memset(qdil[i][1][0:64, :], 0.0), [], [qdilb[i]])

            def load_kq(kind, idx, bi):
                kT, kTb, qT, qTb = kTs[bi], kTbs[bi], qTs[bi], qTbs[bi]
                if kind == "dil":
                    load("sp", kT[:, 0:4096], dkT_all[idx, :, :], [kTb])
                    load("sp", qdil[bi][0][0:64, :], dqT_all[idx, 0:64, :], [qdilb[bi]])
                    load("sp", qdil[bi][1][64:128, :], dqT_all[idx, 64:128, :], [qdilb[bi]])
                else:
                    h = idx
                    for r in range(4):
                        load("sp", kT[0:96, r * OWN:(r + 1) * OWN], kT_g[h // 2][r * 192 + (h % 2) * 96: r * 192 + (h % 2 + 1) * 96, :], [kTb], [kgb])
                    load("sp", qT[0:96, :], qT_all[h, :, :], [qTb])

            def load_v(kind, h, bi):
                vv, vvb = vvs[bi], vvbs[bi]
                if kind == "dil":
                    for j0 in range(0, 32, 16):
                        load("sp", vv[:, j0:j0 + 16, :], dvaug_all.ap()[j0 * 128:(j0 + 16) * 128, h * 65:(h + 1) * 65].rearrange("(j p) c -> p j c", p=128), [vvb])
                else:
                    for r in range(4):
                        for c4 in range(4):
                            j0 = r * 16 + c4 * 4
                            load("sp", vv[:, j0:j0 + 4, :], vaug_all[c4].ap()[r * 512:(r + 1) * 512, h * 65:(h + 1) * 65].rearrange("(j p) c -> p j c", p=128), [vvb], [vgb])

            jobs = [("dil", h) for h in range(8)] + [("mla", h) for h in range(8)]
            kq_slot = {}
            nkq = 0

            def issue_loads(ji):
                nonlocal_kq = kq_slot
                kind, h = jobs[ji]
                key = (kind, h // 2) if kind == "dil" else (kind, h)
                if key not in nonlocal_kq:
                    nonlocal_kq[key] = len(nonlocal_kq) % 2
                    load_kq(kind, key[1], nonlocal_kq[key])
                load_v(kind, h, ji % 2)

            kq_slot[("dil", 0)] = 0
            load_kq("dil", 0, 0)
            load("sp", msk[:, 0:5, :], masks_d[:, 0:5, :], [mskbs[0]])
            load_v("dil", 0, 0)
            for c5 in range(1, 4):
                load("sp", msk[:, c5 * 5:(c5 + 1) * 5, :], masks_d[:, c5 * 5:(c5 + 1) * 5, :], [mskbs[c5]])
            wsrc = {"g": wg_d, "u": wu_d, "d": wd_d}
            precast = [(m, ex) for ex in range(NE) for m in "gud"]
            pci = [0]
            for ji, (kind, h) in enumerate(jobs):
                if ji + 1 < len(jobs):
                    issue_loads(ji + 1)
                key = (kind, h // 2) if kind == "dil" else (kind, h)
                bi = kq_slot[key]
                for qb in range(NO):
                    if kind == "dil":
                        hh = h % 2
                        attend(kTs[bi], kTbs[bi], qdil[bi][hh], qdilb[bi], vvs[ji % 2], vvbs[ji % 2], 20, lambda kt, qb=qb: qb * 512 + kt * 128, lambda k0, qb=qb: qb * 4 + k0,
                               0, 128, 0.125, True, 512 + h * 64, qb, blk)
                    else:
                        attend(kTs[bi], kTbs[bi], qTs[bi], qTbs[bi], vvs[ji % 2], vvbs[ji % 2], 64, lambda kt: kt * 128, lambda k0: k0, 0, 96, 96 ** -0.5, False, h * 64, qb, blk)
                    blk += 1
                    if blk >= 8 and not (24 <= blk < 32) and pci[0] < len(precast):
                        m, ex = precast[pci[0]]; pci[0] += 1
                        sc.dma("pool", lambda e, m=m, ex=ex: e.dma_start(out=w16[m][ex, :, :], in_=wsrc[m][ex, :, :]), [pace], [preb[(m, ex)]], ring="pre")
            for c in range(8):
                sc.op("dve", lambda e, c=c: e.tensor_tensor(out=wo[:, c, :], in0=wo[:, c, :], in1=gtab[:], op=ALU.mult), [wob, cb], [wob])
            ps.pop()
        sc.barrier()

        yaccb = Buf(); h2ob = Buf()
        with contextlib.ExitStack() as p3:
            wr = sb(p3, "wr", [128, 8, NE]); brb = sb(p3, "brb", [128, NE]); w3b = Buf()
            load("sp", wr[:], wr_d.ap().rearrange("(k p) n -> p k n", p=128), [w3b])
            load("sp", brb[:], br_d.ap().partition_broadcast(128), [w3b])
            mg = sb(p3, "mg", [128, 8, 512], BF16); mgb = Buf()
            xts = [sb(p3, f"xt{i}", [128, 1024]) for i in range(2)]; xtb = [Buf(), Buf()]
            x1s = [sb(p3, f"x1{i}", [128, 1024]) for i in range(2)]; x1b = [Buf(), Buf()]
            h2s = [sb(p3, f"h2{i}", [128, 1024]) for i in range(2)]; h2b = [Buf(), Buf()]
            junk = sb(p3, "junk", [128, 1024]); junkb = Buf()
            h2T = sb(p3, "h2T", [128, 8, 128]); h2Tb = Buf()
            sms = [sb(p3, f"sm{i}", [128, 8]) for i in range(2)]; smb = [Buf(), Buf()]
            lg = sb(p3, "lg", [128, NE]); lgb = Buf()
            mgs = [mg, sb(p3, "mg2", [128, 8, 512], BF16)]; mgbs = [mgb, Buf()]

            def stage_a(tt):
                g, t = tt // 4, tt % 4
                r0 = tt * 128
                i = tt % 2
                xt, x1, h2, sm = xts[i], x1s[i], h2s[i], sms[i]
                mgc, mgcb = mgs[g % 2], mgbs[g % 2]
                if t == 0 and g == 0:
                    load("sp", mgc[:], mixT_all.ap()[:, g * 512:(g + 1) * 512].rearrange("(c p) t -> p c t", p=128), [mgcb])
                if t == 0 and g + 1 < NO:
                    load("sp", mgs[(g + 1) % 2][:], mixT_all.ap()[:, (g + 1) * 512:(g + 2) * 512].rearrange("(c p) t -> p c t", p=128), [mgbs[(g + 1) % 2]])
                load("sp", xt[:], x_d[1024 + r0:1024 + r0 + 128, :], [xtb[i]])
                for dh in range(2):
                    for c in range(8):
                        sc.op("pe", lambda e, dh=dh, c=c, t=t: e.matmul(ps[dh][:, :], lhsT=mgc[:, c, t * 128:(t + 1) * 128], rhs=wo[:, c, dh * 512:(dh + 1) * 512],
                                                                      start=(c == 0), stop=(c == 7)), [mgcb, wob], [psb[dh]])
                    sc.op("dve", lambda e, dh=dh, x1=x1, xt=xt: e.tensor_tensor(out=x1[:, dh * 512:(dh + 1) * 512], in0=ps[dh][:, :], in1=xt[:, dh * 512:(dh + 1) * 512], op=ALU.add),
                          [psb[dh], xtb[i]], [x1b[i]])
                sc.dma("pool", lambda e, r0=r0, x1=x1: e.dma_start(out=yacc[r0:r0 + 128, :], in_=x1[:]), [x1b[i]], [yaccb])
                sc.op("act", lambda e, x1=x1, sm=sm: e.activation(out=junk[:], in_=x1[:], func=AF.Square, accum_out=sm[:, 0:1]), [x1b[i]], [junkb, smb[i]])
                sc.op("dve", lambda e, sm=sm: e.tensor_scalar(out=sm[:, 1:2], in0=sm[:, 0:1], scalar1=1.0 / D, scalar2=EPS, op0=ALU.mult, op1=ALU.add), [smb[i]], [smb[i]])
                sc.op("act", lambda e, sm=sm: e.activation(out=sm[:, 1:2], in_=sm[:, 1:2], func=AF.Ln), [smb[i]], [smb[i]])
                sc.op("act", lambda e, sm=sm: e.activation(out=sm[:, 2:3], in_=sm[:, 1:2], func=AF.Exp, scale=-0.5), [smb[i]], [smb[i]])
                sc.op("dve", lambda e, sm=sm, h2=h2, x1=x1: e.scalar_tensor_tensor(out=h2[:], in0=x1[:], scalar=sm[:, 2:3], in1=g2b[:], op0=ALU.mult, op1=ALU.mult), [x1b[i], smb[i], cb], [h2b[i]])
                sc.op("dve", lambda e, h2=h2: e.tensor_tensor(out=h2[:], in0=h2[:], in1=shmb[:], op=ALU.add), [h2b[i], cb], [h2b[i]])
                sc.dma("pool", lambda e, r0=r0, h2=h2: e.dma_start(out=h2_all[r0:r0 + 128, :], in_=h2[:]), [h2b[i]], [h2ob])

            def stage_b(tt):
                r0 = tt * 128
                i = tt % 2
                h2, sm = h2s[i], sms[i]
                for half in range(2):
                    for kk in range(4):
                        k = half * 4 + kk
                        sc.op("pe", lambda e, k=k, kk=kk, half=half, h2=h2: e.transpose(out=ps[2 + half][:, kk * 128:(kk + 1) * 128], in_=h2[:, k * 128:(k + 1) * 128], identity=identf[:]),
                              [h2b[i], cb], [psb[2 + half]])
                    sc.op("act", lambda e, half=half: e.activation(out=h2T[:, half * 4:(half + 1) * 4, :], in_=ps[2 + half][:, :].rearrange("p (k t) -> p k t", k=4), func=AF.Copy),
                          [psb[2 + half]], [h2Tb])
                for k in range(8):
                    sc.op("pe", lambda e, k=k: e.matmul(ps[4][:, 0:NE], lhsT=h2T[:, k, :], rhs=wr[:, k, :], start=(k == 0), stop=(k == 7)), [h2Tb, w3b], [psb[4]])
                sc.op("dve", lambda e: e.tensor_tensor(out=lg[:], in0=ps[4][:, 0:NE], in1=brb[:], op=ALU.add), [psb[4], w3b], [lgb])
                sc.op("dve", lambda e, sm=sm: e.reduce_max(out=sm[:, 3:4], in_=lg[:], axis=mybir.AxisListType.X), [lgb], [smb[i]])
                sc.op("dve", lambda e, sm=sm: e.tensor_scalar(out=sm[:, 4:5], in0=sm[:, 3:4], scalar1=-1.0, scalar2=None, op0=ALU.mult), [smb[i]], [smb[i]])
                sc.op("act", lambda e, sm=sm: e.activation(out=lg[:], in_=lg[:], func=AF.Exp, bias=sm[:, 4:5], scale=1.0, accum_out=sm[:, 5:6]), [lgb, smb[i]], [lgb, smb[i]])
                sc.op("dve", lambda e, sm=sm: e.reciprocal(out=sm[:, 6:7], in_=sm[:, 5:6]), [smb[i]], [smb[i]])
                sc.op("dve", lambda e, sm=sm, tt=tt: e.tensor_scalar(out=affo[:, tt, :], in0=lg[:], scalar1=sm[:, 6:7], scalar2=None, op0=ALU.mult), [lgb, smb[i]], [affob])
                sc.dma("pool", lambda e, r0=r0, tt=tt: e.dma_start(out=aff_send[r0:r0 + 128, :], in_=affo[:, tt, :]), [affob], [affsb])

            stage_a(0)
            for tt in range(16):
                if tt + 1 < 16:
                    stage_a(tt + 1)
                stage_b(tt)
            sc.coll(es, lambda e: e.collective_compute("AllGather", ALU.bypass, replica_groups=[[0, 1, 2, 3], [4, 5, 6, 7]],
                                                       ins=[aff_send.ap().opt()], outs=[aff_all.ap().opt()]), [affsb], [affgb])
        sc.barrier()
        pwo.close()

        with contextlib.ExitStack() as p4:
            wgs = [sb(p4, f"wg{i}", [128, 8, 1024], BF16) for i in range(2)]
            wus = [sb(p4, f"wu{i}", [128, 8, 1024], BF16) for i in range(2)]
            wds = [sb(p4, f"wd{i}", [128, 8, 1024], BF16) for i in range(2)]
            wgb = [Buf(), Buf()]; wub = [Buf(), Buf()]; wdb = [Buf(), Buf()]

            def load_expert(ex_):
                i = ex_ % 2
                q4 = ex_ // 4
                load("sp", wgs[i][:, :, :], w16["g"].ap()[ex_].rearrange("(k p) n -> p k n", p=128), [wgb[i]], [preb[("g", ex_)]])
                load("sp", wus[i][:, :, :], w16["u"].ap()[ex_].rearrange("(k p) n -> p k n", p=128), [wub[i]], [preb[("u", ex_)]])
                load("sp", wds[i][:, :, :], w16["d"].ap()[ex_].rearrange("(k p) n -> p k n", p=128), [wdb[i]], [preb[("d", ex_)]])

            ps.append(p4.enter_context(nc.psum_tensor("ps6b", [128, 512], F32)))
            pst = p4.enter_context(nc.psum_tensor("pstB", [128, 1024], BF16))
            load_expert(0); load_expert(1)
            aff = sb(p4, "aff", [128, 64, NE]); affb = Buf()
            cmp = sb(p4, "cmp", [128, 64, NE]); cmpb = Buf()
            cntp = sb(p4, "cntp", [128, NE]); cntb = Buf()
            lo = sb(p4, "lo", [128, NE]); hi = sb(p4, "hi", [128, NE]); mid = sb(p4, "mid", [128, NE]); ge = sb(p4, "ge", [128, NE]); dlt = sb(p4, "dlt", [128, NE])
            bsb_ = Buf()
            load("sp", aff[:], aff_all.ap().rearrange("(p j) e -> p j e", p=128), [affb], [affgb])
            sc.op("dve", lambda e: e.memset(lo[:], 0.0), [], [bsb_])
            sc.op("dve", lambda e: e.memset(hi[:], 1.0), [], [bsb_])
            sc.op("dve", lambda e: e.memset(mid[:], 0.5), [], [bsb_])
            for it in range(N_BISECT):
                sc.op("dve", lambda e: e.tensor_tensor(out=cmp[:], in0=aff[:], in1=mid[:].rearrange("p (o e) -> p o e", o=1).broadcast_to([128, 64, NE]), op=ALU.is_gt), [affb, bsb_], [cmpb])
                sc.op("dve", lambda e: e.tensor_reduce(out=cntp[:], in_=cmp[:].rearrange("p j e -> p e j"), axis=mybir.AxisListType.X, op=ALU.add), [cmpb], [cntb])
                sc.op("pe", lambda e: e.matmul(ps[0][:, 0:NE], lhsT=onesf[:], rhs=cntp[:], start=True, stop=True), [cntb, cb], [psb[0]])
                sc.op("dve", lambda e: e.tensor_scalar(out=ge[:], in0=ps[0][:, 0:NE], scalar1=1024.0, scalar2=None, op0=ALU.is_ge), [psb[0]], [bsb_])
                sc.op("dve", lambda e: e.tensor_tensor(out=dlt[:], in0=mid[:], in1=lo[:], op=ALU.subtract), [bsb_], [bsb_])
                sc.op("dve", lambda e: e.tensor_tensor(out=dlt[:], in0=dlt[:], in1=ge[:], op=ALU.mult), [bsb_], [bsb_])
                sc.op("dve", lambda e: e.tensor_tensor(out=lo[:], in0=lo[:], in1=dlt[:], op=ALU.add), [bsb_], [bsb_])
                sc.op("dve", lambda e: e.tensor_tensor(out=dlt[:], in0=hi[:], in1=mid[:], op=ALU.subtract), [bsb_], [bsb_])
                sc.op("dve", lambda e: e.tensor_tensor(out=dlt[:], in0=dlt[:], in1=ge[:], op=ALU.mult), [bsb_], [bsb_])
                sc.op("dve", lambda e: e.tensor_tensor(out=hi[:], in0=mid[:], in1=dlt[:], op=ALU.add), [bsb_], [bsb_])
                sc.op("dve", lambda e: e.tensor_tensor(out=mid[:], in0=lo[:], in1=hi[:], op=ALU.add), [bsb_], [bsb_])
                sc.op("dve", lambda e: e.tensor_scalar(out=mid[:], in0=mid[:], scalar1=0.5, scalar2=None, op0=ALU.mult), [bsb_], [bsb_])

            trit = sb(p4, "trit", [128, 128]); trib = sb(p4, "trib", [128, 128], BF16)
            iot = sb(p4, "iot", [128, CAP]); slotc = sb(p4, "slotc", [128, 3]); ob = Buf()
            load("sp", trit[:], tri_d[:, :], [ob]); load("sp", iot[:], iota_d[:, :], [ob]); load("sp", slotc[:], slotc_d[:, :], [ob])
            sc.op("dve", lambda e: e.tensor_copy(out=trib[:], in_=trit[:]), [ob], [ob])
            mk = sb(p4, "mk", [128, 16, NE], BF16); mkb = Buf()
            cc = sb(p4, "cc", [128, 16, NE]); ccb = Buf()
            ex = sb(p4, "ex", [128, 16, NE]); exb = Buf()
            sc.op("dve", lambda e: e.tensor_tensor(out=mk[:], in0=affo[:], in1=lo[:].rearrange("p (o e) -> p o e", o=1).broadcast_to([128, 16, NE]), op=ALU.is_gt), [affob, bsb_], [mkb])
            sc.op("pe", lambda e: e.matmul(ps[0][:, 0:256], lhsT=trib[:], rhs=mk[:].rearrange("p j e -> p (j e)"), start=True, stop=True), [mkb, ob], [psb[0]])
            sc.op("pe", lambda e: e.matmul(ps[1][:, 0:256], lhsT=onesb[:], rhs=mk[:].rearrange("p j e -> p (j e)"), start=True, stop=True), [mkb, cb], [psb[1]])
            sc.op("dve", lambda e: e.tensor_copy(out=cc[:].rearrange("p j e -> p (j e)"), in_=ps[0][:, 0:256]), [psb[0]], [ccb])
            sc.op("dve", lambda e: e.memset(ex[:, 0, :], 0.0), [], [exb])
            for j in range(1, 16):
                sc.op("dve", lambda e, j=j: e.tensor_tensor(out=ex[:, j, :], in0=ex[:, j - 1, :], in1=ps[1][:, (j - 1) * NE:j * NE], op=ALU.add), [exb, psb[1]], [exb])
            sc.op("dve", lambda e: e.tensor_tensor(out=cc[:], in0=cc[:], in1=ex[:], op=ALU.add), [ccb, exb], [ccb])
            lt = [sb(p4, f"lt{i}", [128, CAP], BF16) for i in range(16)]; ltb = [Buf() for _ in range(16)]
            idxf = sb(p4, "idxf", [128, 3, NE]); idxb = Buf()
            gidx = sb(p4, "gidx", [128, 3, NE], I32); sidx = sb(p4, "sidx", [128, 3, NE], I32); tmpi = sb(p4, "tmpi", [128, 3, NE])
            idxbs = [Buf() for _ in range(NE)]
            slot3 = slotc[:].rearrange("p (s o) -> p s o", o=1)

            def idx_dve(ex_):
                for j in range(16):
                    sc.op("dve", lambda e, j=j, ex_=ex_: e.tensor_scalar(out=lt[j][:], in0=iot[:], scalar1=cc[:, j, ex_:ex_ + 1], scalar2=None, op0=ALU.is_ge),
                          [ob, ccb], [ltb[j]])

            def idx_pe(ex_):
                for st in range(3):
                    for j in range(16):
                        sc.op("pe", lambda e, j=j, ex_=ex_, st=st: e.matmul(ps[5][:, st * NE + ex_: st * NE + ex_ + 1], lhsT=lt[j][:, st * 128:(st + 1) * 128],
                                                                           rhs=onesb[:, 0:1], start=(j == 0), stop=(j == 15)), [ltb[j], cb], [psb[5]])
                ib = idxbs[ex_]
                sl = slice(ex_, ex_ + 1)
                sc.op("dve", lambda e: e.tensor_copy(out=idxf[:, :, sl], in_=ps[5][:, 0:3 * NE].rearrange("p (s e) -> p s e", s=3)[:, :, sl]), [psb[5]], [ib])
                sc.op("dve", lambda e: e.tensor_scalar(out=tmpi[:, :, sl], in0=idxf[:, :, sl], scalar1=2047.0, scalar2=None, op0=ALU.min), [ib, ob], [ib])
                sc.op("dve", lambda e: e.tensor_copy(out=gidx[:, :, sl], in_=tmpi[:, :, sl]), [ib], [ib])
                sc.op("dve", lambda e: e.tensor_scalar(out=tmpi[:, :, sl], in0=idxf[:, :, sl], scalar1=2048.0, scalar2=None, op0=ALU.is_ge), [ib], [ib])
                sc.op("dve", lambda e: e.tensor_tensor(out=tmpi[:, :, sl], in0=tmpi[:, :, sl], in1=slot3, op=ALU.mult), [ib, ob], [ib])
                sc.op("dve", lambda e: e.tensor_tensor(out=tmpi[:, :, sl], in0=tmpi[:, :, sl], in1=idxf[:, :, sl], op=ALU.add), [ib], [ib])
                sc.op("dve", lambda e: e.tensor_copy(out=sidx[:, :, sl], in_=tmpi[:, :, sl]), [ib], [ib])

            xe = [sb(p4, f"xe{i}", [128, 1024], BF16) for i in range(6)]; xeb = [Buf() for _ in range(6)]
            gat = [sb(p4, f"gat{i}", [128, NE]) for i in range(6)]; gatb = [Buf() for _ in range(6)]
            xeT = sb(p4, "xeT", [128, 8, CAP], BF16); xeTb = Buf()
            hh_ = sb(p4, "hh", [128, 8, CAP], BF16); hhb = Buf()
            sils = [sb(p4, f"sil{i}", [128, CAP]) for i in range(2)]; silb = [Buf(), Buf()]
            ye = [sb(p4, f"ye{i}", [128, 1024]) for i in range(2)]; yeb = [Buf(), Buf()]

            def gathers(ex_):
                for st in range(3):
                    xi = (ex_ % 2) * 3 + st
                    sc.dma("pool", lambda e, st=st, ex_=ex_, xi=xi: e.indirect_dma_start(out=xe[xi][:, :], out_offset=None, in_=h2_all[:, :],
                                                                                         in_offset=bass.IndirectOffsetOnAxis(ap=gidx[:, st, ex_:ex_ + 1], axis=0)), [idxbs[ex_], h2ob], [xeb[xi]])
                    sc.dma("pool", lambda e, st=st, ex_=ex_, xi=xi: e.indirect_dma_start(out=gat[xi][:, :], out_offset=None, in_=aff_send[:, :],
                                                                                         in_offset=bass.IndirectOffsetOnAxis(ap=gidx[:, st, ex_:ex_ + 1], axis=0)), [idxbs[ex_], affsb], [gatb[xi]])

            idx_dve(0); idx_pe(0); gathers(0)
            idx_dve(1); idx_pe(1); gathers(1)
            yn = 0
            fcn = 0
            for ex_ in range(NE):
                i = ex_ % 2
                if ex_ + 2 < NE:
                    idx_dve(ex_ + 2)
                for st in range(3):
                    xi = i * 3 + st
                    for k in range(8):
                        sc.op("pe", lambda e, xi=xi, k=k: e.transpose(out=pst[:, k * 128:(k + 1) * 128], in_=xe[xi][:, k * 128:(k + 1) * 128], identity=identb[:]),
                              [xeb[xi], cb], [pstb])
                    sc.op("act", lambda e, st=st: e.activation(out=xeT[:, :, st * 128:(st + 1) * 128], in_=pst[:, :].rearrange("p (k t) -> p k t", k=8), func=AF.Copy),
                          [pstb], [xeTb])
                for fc in range(8):
                    ba, bb = (0, 1) if fcn % 2 == 0 else (2, 6)
                    si = fcn % 2; fcn += 1
                    for k in range(8):
                        sc.op("pe", lambda e, fc=fc, k=k, i=i, ba=ba: e.matmul(ps[ba][:, 0:CAP], lhsT=wgs[i][:, k, fc * 128:(fc + 1) * 128], rhs=xeT[:, k, :], start=(k == 0), stop=(k == 7)),
                              [wgb[i], xeTb], [psb[ba]])
                    for k in range(8):
                        sc.op("pe", lambda e, fc=fc, k=k, i=i, bb=bb: e.matmul(ps[bb][:, 0:CAP], lhsT=wus[i][:, k, fc * 128:(fc + 1) * 128], rhs=xeT[:, k, :], start=(k == 0), stop=(k == 7)),
                              [wub[i], xeTb], [psb[bb]])
                    sc.op("act", lambda e, ba=ba, si=si: e.activation(out=sils[si][:], in_=ps[ba][:, 0:CAP], func=AF.Silu), [psb[ba]], [silb[si]])
                    sc.op("dve", lambda e, fc=fc, bb=bb, si=si: e.tensor_tensor(out=hh_[:, fc, :], in0=sils[si][:], in1=ps[bb][:, 0:CAP], op=ALU.mult), [silb[si], psb[bb]], [hhb])
                for st in range(3):
                    xi = i * 3 + st
                    yi = yn % 2; yn += 1
                    for dh in range(2):
                        for fc in range(8):
                            sc.op("pe", lambda e, st=st, dh=dh, fc=fc, i=i: e.matmul(ps[3 + dh][:, :], lhsT=hh_[:, fc, st * 128:(st + 1) * 128], rhs=wds[i][:, fc, dh * 512:(dh + 1) * 512],
                                                                                    start=(fc == 0), stop=(fc == 7)), [hhb, wdb[i]], [psb[3 + dh]])
                        sc.op("dve", lambda e, xi=xi, dh=dh, yi=yi, ex_=ex_: e.scalar_tensor_tensor(out=ye[yi][:, dh * 512:(dh + 1) * 512], in0=ps[3 + dh][:, :], scalar=gat[xi][:, ex_:ex_ + 1],
                                                                                                  in1=gtmb[:, dh * 512:(dh + 1) * 512], op0=ALU.mult, op1=ALU.mult),
                              [psb[3 + dh], gatb[xi], cb], [yeb[yi]])
                    sc.dma("pool", lambda e, st=st, yi=yi, ex_=ex_: e.indirect_dma_start(out=yacc[:, :], out_offset=bass.IndirectOffsetOnAxis(ap=sidx[:, st, ex_:ex_ + 1], axis=0),
                                                                                         in_=ye[yi][:, :], in_offset=None, compute_op=ALU.add), [yeb[yi], idxbs[ex_]], [yaccb])
                if ex_ + 2 < NE:
                    idx_pe(ex_ + 2)
                    gathers(ex_ + 2)
                    load_expert(ex_ + 2)
            fo = [sb(p4, f"fo{i}", [128, 1024]) for i in range(4)]; fob = [Buf() for _ in range(4)]
            fj = sb(p4, "fj", [128, 1024]); fjb = Buf()
            fss = [sb(p4, f"fs{i}", [128, 4]) for i in range(4)]; fsbs = [Buf() for _ in range(4)]
            for j in range(16):
                i = j % 4
                fs, fsb = fss[i], fsbs[i]
                sc.dma("sp", lambda e, j=j, i=i: e.dma_start(out=fo[i][:], in_=yacc[j * 128:(j + 1) * 128, :]), [yaccb], [fob[i]])
                sc.op("act", lambda e, i=i, fs=fs: e.activation(out=fj[:], in_=fo[i][:], func=AF.Square, accum_out=fs[:, 0:1]), [fob[i]], [fjb, fsb])
                sc.op("dve", lambda e, fs=fs: e.tensor_scalar(out=fs[:, 1:2], in0=fs[:, 0:1], scalar1=1.0 / D, scalar2=EPS, op0=ALU.mult, op1=ALU.add), [fsb], [fsb])
                sc.op("act", lambda e, fs=fs: e.activation(out=fs[:, 1:2], in_=fs[:, 1:2], func=AF.Sqrt), [fsb], [fsb])
                sc.op("dve", lambda e, fs=fs: e.reciprocal(out=fs[:, 2:3], in_=fs[:, 1:2]), [fsb], [fsb])
                sc.op("dve", lambda e, i=i, fs=fs: e.scalar_tensor_tensor(out=fo[i][:], in0=fo[i][:], scalar=fs[:, 2:3], in1=gfinb[:], op0=ALU.mult, op1=ALU.mult), [fob[i], fsb, cb], [fob[i]])
                sc.dma("pool", lambda e, j=j, i=i: e.dma_start(out=y_d[j * 128:(j + 1) * 128, :], in_=fo[i][:]), [fob[i]], [])
            sc.barrier(final=True)
    return nc


def _rope_cols():
    rc = np.zeros((128, 8), np.float32)
    inv_m = (1.0 / (500000.0 ** (np.arange(0, 32, 2, dtype=np.float32) / 32))).astype(np.float32)
    inv_d = (1.0 / (500000.0 ** (np.arange(0, 16, 2, dtype=np.float32) / 16))).astype(np.float32)
    hp = np.float32(np.pi / 2)
    rc[:, 1] = hp
    rc[64:80, 0] = inv_m; rc[80:96, 0] = inv_m
    rc[64:80, 2] = -inv_m; rc[80:96, 2] = inv_m
    rc[:, 5] = hp
    for base in (0, 64):
        rc[base:base + 8, 4] = inv_d; rc[base + 8:base + 16, 4] = inv_d
        rc[base:base + 8, 6] = -inv_d; rc[base + 8:base + 16, 6] = inv_d
    return rc


def _masks():
    k = np.arange(128)[:, None]
    q = np.arange(512)[None, :]
    out = np.zeros((128, 20, 512), np.float32)
    for j in range(20):
        rel = 128 * j - 1024 + k - q
        m = np.zeros((128, 512), np.float32)
        for r in (1, 4, 16):
            m += ((rel % r == 0) & (np.abs(rel) <= 64 * r)).astype(np.float32)
        out[:, j, :] = np.where(m > 0, 8.0 * np.log(np.maximum(m, 1.0)), -30000.0)
    return out.astype(ml_dtypes.bfloat16)


def _col(v, n):
    return np.ascontiguousarray(np.asarray(v, np.float32).reshape(n, 128).T)


_NC_CACHE = {}


def kernel(x, c, positions, w_ada, b_ada, g_mix, w_in, g_q, w_uq, g_kv, w_ukv, w_out,
           g_ffn, w_router, b_router, w_gate, w_up, w_down, g_final):
    x = np.asarray(x, np.float32); c = np.asarray(c, np.float32)
    positions = np.asarray(positions, np.int32)
    w_in0 = np.asarray(w_in, np.float32)[0]
    o3 = 672
    dq = w_in0[:, o3:o3 + 512]; dk = w_in0[:, o3 + 512:o3 + 1024]; dv = w_in0[:, o3 + 1024:o3 + 1536]
    kr = w_in0[:, 640:672]

    def swap_heads(w, hd, rot):
        w = w.reshape(D, -1, hd).copy()
        s = w.copy()
        s[:, :, 0:rot // 2] = w[:, :, rot // 2:rot]
        s[:, :, rot // 2:rot] = w[:, :, 0:rot // 2]
        return s.reshape(D, -1)

    win_l = np.zeros((D, WI_COLS), np.float32)
    win_l[:, CQ:CQ + 384] = w_in0[:, 0:384]
    win_l[:, CKV:CKV + 256] = w_in0[:, 384:640]
    win_l[:, DQ:DQ + 512] = dq; win_l[:, DQS:DQS + 512] = swap_heads(dq, 64, 16)
    win_l[:, DK:DK + 512] = dk; win_l[:, DKS:DKS + 512] = swap_heads(dk, 64, 16)
    win_l[:, DV:DV + 512] = dv
    win_l[:, KR + 64:KR + 96] = kr
    win_l[:, KRS + 64:KRS + 80] = kr[:, 16:32]; win_l[:, KRS + 80:KRS + 96] = kr[:, 0:16]
    wuq0 = np.asarray(w_uq, np.float32)[0]
    wuq_s = wuq0.reshape(384, 8, 96).copy()
    tmp = wuq_s.copy()
    wuq_s[:, :, 64:80] = tmp[:, :, 80:96]; wuq_s[:, :, 80:96] = tmp[:, :, 64:80]
    wuq_l = np.ascontiguousarray(np.concatenate([wuq0, wuq_s.reshape(384, 768)], axis=1))
    wukv0 = np.asarray(w_ukv, np.float32)[0].reshape(256, 8, 128)
    wukv_k = np.ascontiguousarray(wukv0[:, :, 0:64].reshape(256, 512))
    wukv_v = np.ascontiguousarray(wukv0[:, :, 64:128].reshape(256, 512))
    sel = np.zeros((128, 64), np.float32); sel[64, :] = 1.0
    tri = (np.arange(128)[:, None] <= np.arange(128)[None, :]).astype(np.float32)
    iota_s = np.broadcast_to(np.arange(CAP, dtype=np.float32), (128, CAP)).copy()
    slot_col = (np.arange(3)[None, :] * 128 + np.arange(128)[:, None]).astype(np.float32)
    shared = {
        "g_mix_col": _col(np.asarray(g_mix)[0], 8),
        "g_ffn_col": _col(np.asarray(g_ffn)[0], 8),
        "g_final": np.asarray(g_final, np.float32),
        "w_in_l": win_l,
        "g_q_col": _col(np.asarray(g_q)[0], 3),
        "g_kv_col": _col(np.asarray(g_kv)[0], 2),
        "w_uq_l": wuq_l, "w_ukv_k": wukv_k, "w_ukv_v": wukv_v,
        "w_out": np.ascontiguousarray(np.asarray(w_out, np.float32)[0]),
        "w_router": np.ascontiguousarray(np.asarray(w_router, np.float32)[0]),
        "b_router": np.ascontiguousarray(np.asarray(b_router, np.float32)[0]),
        "w_gate": np.ascontiguousarray(np.asarray(w_gate, np.float32)[0]),
        "w_up": np.ascontiguousarray(np.asarray(w_up, np.float32)[0]),
        "w_down": np.ascontiguousarray(np.asarray(w_down, np.float32)[0]),
        "ident_f": np.eye(128, dtype=np.float32), "tri": tri, "sel": sel, "iota_s": iota_s, "slot_col": slot_col,
        "masks": _masks(), "rope_cols": _rope_cols(),
            }
    wada0 = np.asarray(w_ada, np.float32)[0]
    bada0 = np.asarray(b_ada, np.float32)[0]
    in_maps = []
    for core in range(NCORES):
        b, qtr = core // 4, core % 4
        m = dict(shared)
        lo_t = qtr * OWN - 1024
        xw = np.zeros((4096, D), np.float32)
        pw = np.zeros((4096,), np.int32)
        vw = np.zeros((4096,), np.float32)
        s0, s1 = max(lo_t, 0), min(lo_t + 4096, S)
        xw[s0 - lo_t:s1 - lo_t] = x[b, s0:s1]
        pw[s0 - lo_t:s1 - lo_t] = positions[b, s0:s1]
        vw[s0 - lo_t:s1 - lo_t] = 1.0
        m["x_win"] = xw
        m["pos_win"] = pw
        m["valid_col"] = np.ascontiguousarray(vw.reshape(32, 128).T)
        m["c_col"] = _col(c[b], 8)
        m["w_ada_q"] = np.ascontiguousarray(wada0[:, qtr * 1536:(qtr + 1) * 1536])
        m["b_ada_colq"] = _col(bada0[qtr * 1536:(qtr + 1) * 1536], 12)
        in_maps.append(m)
    if "nc" not in _NC_CACHE:
        _NC_CACHE["nc"] = build_program()
    res = run_bass_kernel_spmd(_NC_CACHE["nc"], in_maps, core_ids=list(range(NCORES)))
    _NC_CACHE["last"] = res.results
    out = np.zeros((2, S, D), np.float32)
    for core in range(NCORES):
        b, qtr = core // 4, core % 4
        out[b, qtr * OWN:(qtr + 1) * OWN] = np.asarray(res.results[core]["y"], np.float32)
    return out


if __name__ == "__main__":
    import time
    t0 = time.time()
    nc = build_program()
    print("build ok", time.time() - t0)
```
